# Optimizing a Trainium2 kernel written in Bass

```python
import math
import jax, jax.numpy as jnp
from jax import lax
import numpy as np

D_MODEL = 1024
BATCH = 16
SEQ = 4096
DEPTH = 4

D_MIX = D_MODEL
HG_WIDTH = D_MIX // 2
HG_HEAD_DIM = 128
HG_HEADS = HG_WIDTH // HG_HEAD_DIM
HG_CHUNK = 64
NSA_WIDTH = D_MIX - HG_WIDTH
NSA_HEAD_DIM = 64
NSA_HEADS = NSA_WIDTH // NSA_HEAD_DIM
NSA_KV_GROUPS = 2
NSA_HPG = NSA_HEADS // NSA_KV_GROUPS
CMP_BLOCK = 32
CMP_STRIDE = 16
CMP_HIDDEN = 128
SLC_BLOCK = 64
SLC_TOPK = 16
SLC_LOCAL = 2
WINDOW = 512
Q_BLOCK = 64
N_BUCKETS = 32
MAX_DISTANCE = 128
D_FF = ((8 * D_MODEL // 3 + 255) // 256) * 256
ALPHA = (2.0 * DEPTH) ** 0.25
BETA = (8.0 * DEPTH) ** -0.25
KV_W = NSA_KV_GROUPS * NSA_HEAD_DIM
SPLIT_SIZES = (HG_WIDTH, HG_WIDTH, HG_WIDTH, HG_WIDTH, NSA_WIDTH, KV_W, KV_W, KV_W, KV_W, KV_W, KV_W, 3 * NSA_HEADS)
D_IN = sum(SPLIT_SIZES)
NEG = -1e30
BIG = 1e9

kernel_name = "hgrn2_nsa_parallel_heads_deepnorm"


def _split_points():
    pts, acc = [], 0
    for s in SPLIT_SIZES[:-1]:
        acc += s
        pts.append(acc)
    return pts


def layer_norm(x, g, b, eps=1e-5):
    xf = x.astype(jnp.float32)
    mu = jnp.mean(xf, -1, keepdims=True)
    var = jnp.mean(jnp.square(xf - mu), -1, keepdims=True)
    return ((xf - mu) * lax.rsqrt(var + eps) * g + b).astype(x.dtype)


def hgrn2_lower_bounds(lb_param):
    p = jax.nn.softmax(lb_param.astype(jnp.float32), axis=0)
    c = jnp.cumsum(p, axis=0)
    return c - c[0:1]


def hgrn2_mixer(q, f_logit, i, g, lb, gnorm_w):
    B, S, _ = q.shape
    H, Dk, C = HG_HEADS, HG_HEAD_DIM, HG_CHUNK
    N = S // C
    f32 = jnp.float32
    logf = jnp.logaddexp(jnp.log(lb), jnp.log1p(-lb) + jax.nn.log_sigmoid(f_logit.astype(f32)))
    k = -jnp.expm1(logf)

    def to_chunks(t):
        return t.astype(f32).reshape(B, N, C, H, -1).transpose(1, 0, 3, 2, 4)

    qc, kc, vc = to_chunks(q), to_chunks(k), to_chunks(i)
    bc = jnp.cumsum(to_chunks(logf), axis=3)
    causal = jnp.tril(jnp.ones((C, C), bool))

    def step(state, xs):
        qb, kb, vb, bb = xs
        rel = bb[:, :, :, None, :] - bb[:, :, None, :, :]
        decay = jnp.exp(jnp.where(causal[:, :, None], rel, -jnp.inf))
        scores = jnp.einsum('bhtd,bhsd,bhtsd->bhts', qb, kb, decay)
        o_intra = jnp.einsum('bhts,bhsv->bhtv', scores, vb)
        o_inter = jnp.einsum('bhtd,bhdv->bhtv', qb * jnp.exp(bb), state)
        b_last = bb[:, :, -1:, :]
        k_dec = kb * jnp.exp(b_last - bb)
        new_state = state * jnp.exp(b_last[:, :, 0, :, None]) + jnp.einsum('bhsd,bhsv->bhdv', k_dec, vb)
        return new_state, o_intra + o_inter

    s0 = jnp.zeros((B, H, Dk, Dk), f32)
    _, o = lax.scan(step, s0, (qc, kc, vc, bc))
    o = o.transpose(1, 0, 3, 2, 4).reshape(B, S, H, Dk)
    o = o * lax.rsqrt(jnp.mean(o * o, -1, keepdims=True) + 1e-6) * gnorm_w
    o = o.reshape(B, S, HG_WIDTH) * jax.nn.silu(g.astype(f32))
    return o.astype(q.dtype)


def t5_bucket(n):
    n = jnp.maximum(n, 0)
    max_exact = N_BUCKETS // 2
    nf = jnp.maximum(n, 1).astype(jnp.float32)
    large = max_exact + (jnp.log(nf / max_exact) / math.log(MAX_DISTANCE / max_exact)
                         * (N_BUCKETS - max_exact)).astype(jnp.int32)
    large = jnp.minimum(large, N_BUCKETS - 1)
    return jnp.where(n < max_exact, n, large)


def masked_softmax(s, valid):
    s = jnp.where(valid, s, NEG)
    p = jax.nn.softmax(s, axis=-1)
    return jnp.where(valid, p, 0.0)


def nsa_mixer(q, k_cmp, v_cmp, k_slc, v_slc, k_win, v_win, gates,
              cmp_pos_k, cmp_w1_k, cmp_w2_k, cmp_pos_v, cmp_w1_v, cmp_w2_v, rel_bias):
    B, S, _ = q.shape
    G, HPG, Dh = NSA_KV_GROUPS, NSA_HPG, NSA_HEAD_DIM
    f32 = jnp.float32
    scale = Dh ** -0.5
    rb = rel_bias.astype(f32)
    qh = q.astype(f32).reshape(B, S, G, HPG, Dh).transpose(0, 2, 3, 1, 4)
    gh = jax.nn.sigmoid(gates.astype(f32)).reshape(B, S, 3, G, HPG).transpose(0, 3, 4, 1, 2)

    def kv(t):
        return t.astype(f32).reshape(B, S, G, Dh).transpose(0, 2, 1, 3)

    n_cmp = (S - CMP_BLOCK) // CMP_STRIDE + 1

    def compress(t, pos, w1, w2):
        c = kv(t).reshape(B, G, S // CMP_STRIDE, CMP_STRIDE, Dh)
        blocks = jnp.concatenate([c[:, :, :-1], c[:, :, 1:]], axis=3)
        blocks = (blocks + pos).reshape(B, G, n_cmp, CMP_BLOCK * Dh)
        return jax.nn.gelu(blocks @ w1) @ w2

    kc = compress(k_cmp, cmp_pos_k, cmp_w1_k, cmp_w2_k)
    vc = compress(v_cmp, cmp_pos_v, cmp_w1_v, cmp_w2_v)
    cmp_start = jnp.arange(n_cmp) * CMP_STRIDE
    cmp_end = cmp_start + CMP_BLOCK - 1

    n_slc = S // SLC_BLOCK
    n_sel = min(SLC_TOPK, n_slc)
    ks_blocks = kv(k_slc).reshape(B, G, n_slc, SLC_BLOCK, Dh)
    vs_blocks = kv(v_slc).reshape(B, G, n_slc, SLC_BLOCK, Dh)
    slc_start = jnp.arange(n_slc) * SLC_BLOCK
    overlap = jnp.clip(jnp.minimum(cmp_start[:, None] + CMP_BLOCK, slc_start[None, :] + SLC_BLOCK)
                       - jnp.maximum(cmp_start[:, None], slc_start[None, :]), 0, None)
    agg = overlap.astype(f32) / CMP_STRIDE

    kw_pad = jnp.pad(kv(k_win), ((0, 0), (0, 0), (WINDOW, 0), (0, 0)))
    vw_pad = jnp.pad(kv(v_win), ((0, 0), (0, 0), (WINDOW, 0), (0, 0)))
    tab_g = rb.reshape(N_BUCKETS, G, HPG)
    bi = jnp.arange(B)[:, None, None, None]
    gi = jnp.arange(G)[None, :, None, None]

    def head_bias(dist):
        bsh = rb[t5_bucket(dist)]
        return bsh.reshape(dist.shape + (G, HPG)).transpose(2, 3, 0, 1)

    def block_fn(qb_idx):
        t0 = qb_idx * Q_BLOCK
        t_pos = t0 + jnp.arange(Q_BLOCK)
        qblk = lax.dynamic_slice_in_dim(qh, t0, Q_BLOCK, axis=3)
        gblk = lax.dynamic_slice_in_dim(gh, t0, Q_BLOCK, axis=3)
        s_c = jnp.einsum('bghtd,bgnd->bghtn', qblk, kc) * scale + head_bias(t_pos[:, None] - cmp_end[None, :])
        valid_c = cmp_end[None, :] <= t_pos[:, None]
        p_c = masked_softmax(s_c, valid_c)
        o_c = jnp.einsum('bghtn,bgnd->bghtd', p_c, vc)
        imp = jnp.einsum('bghtn,nj->bgtj', p_c, agg)
        cur = t_pos // SLC_BLOCK
        j = jnp.arange(n_slc)
        diff = cur[:, None] - j[None, :]
        forced = (j[None, :] == 0) | ((diff >= 0) & (diff < SLC_LOCAL))
        causal_blk = slc_start[None, :] <= t_pos[:, None]
        score = jnp.where(forced, BIG, jnp.where(causal_blk, imp, -BIG))
        _, idx = lax.top_k(score, n_sel)
        L = n_sel * SLC_BLOCK
        ks_g = ks_blocks[bi, gi, idx].reshape(B, G, Q_BLOCK, L, Dh)
        vs_g = vs_blocks[bi, gi, idx].reshape(B, G, Q_BLOCK, L, Dh)
        key_pos = (idx[..., None] * SLC_BLOCK + jnp.arange(SLC_BLOCK)).reshape(B, G, Q_BLOCK, L)
        dist_s = t_pos[None, None, :, None] - key_pos
        bias_s = jax.vmap(lambda tb, bk: tb[bk], in_axes=(1, 1), out_axes=1)(tab_g, t5_bucket(dist_s))
        bias_s = bias_s.transpose(0, 1, 4, 2, 3)
        s_s = jnp.einsum('bghtd,bgtld->bghtl', qblk, ks_g) * scale + bias_s
        p_s = masked_softmax(s_s, (dist_s >= 0)[:, :, None])
        o_s = jnp.einsum('bghtl,bgtld->bghtd', p_s, vs_g)
        kw = lax.dynamic_slice_in_dim(kw_pad, t0, WINDOW + Q_BLOCK, axis=2)
        vw = lax.dynamic_slice_in_dim(vw_pad, t0, WINDOW + Q_BLOCK, axis=2)
        kpos = t0 - WINDOW + jnp.arange(WINDOW + Q_BLOCK)
        dist_w = t_pos[:, None] - kpos[None, :]
        valid_w = (dist_w >= 0) & (dist_w < WINDOW) & (kpos[None, :] >= 0)
        s_w = jnp.einsum('bghtd,bgkd->bghtk', qblk, kw) * scale + head_bias(dist_w)
        p_w = masked_softmax(s_w, valid_w)
        o_w = jnp.einsum('bghtk,bgkd->bghtd', p_w, vw)
        return gblk[..., 0:1] * o_c + gblk[..., 1:2] * o_s + gblk[..., 2:3] * o_w

    o = lax.map(block_fn, jnp.arange(S // Q_BLOCK))
    o = o.transpose(1, 0, 4, 2, 3, 5).reshape(B, S, NSA_WIDTH)
    return o.astype(q.dtype)


def setup_inputs(seed: int = 0) -> dict:
    key = jax.random.key(seed)
    ks = jax.random.split(key, 20)
    f32 = jnp.float32

    def nrm(k, shape, fan_in, scale=1.0):
        return jax.random.normal(k, shape, f32) * (scale * fan_in ** -0.5)

    def small(k, shape, s):
        return jax.random.normal(k, shape, f32) * s

    return {
        "x": jax.random.normal(ks[0], (BATCH, SEQ, D_MODEL), f32),
        "w_in": nrm(ks[1], (DEPTH, D_MODEL, D_IN), D_MODEL),
        "hg_lb_param": small(ks[2], (DEPTH, HG_WIDTH), 0.1),
        "hg_norm_w": 1.0 + small(ks[3], (DEPTH, HG_HEAD_DIM), 0.02),
        "cmp_pos_k": small(ks[4], (DEPTH, CMP_BLOCK, NSA_HEAD_DIM), 0.02),
        "cmp_w1_k": nrm(ks[5], (DEPTH, CMP_BLOCK * NSA_HEAD_DIM, CMP_HIDDEN), CMP_BLOCK * NSA_HEAD_DIM),
        "cmp_w2_k": nrm(ks[6], (DEPTH, CMP_HIDDEN, NSA_HEAD_DIM), CMP_HIDDEN),
        "cmp_pos_v": small(ks[7], (DEPTH, CMP_BLOCK, NSA_HEAD_DIM), 0.02),
        "cmp_w1_v": nrm(ks[8], (DEPTH, CMP_BLOCK * NSA_HEAD_DIM, CMP_HIDDEN), CMP_BLOCK * NSA_HEAD_DIM),
        "cmp_w2_v": nrm(ks[9], (DEPTH, CMP_HIDDEN, NSA_HEAD_DIM), CMP_HIDDEN),
        "rel_bias": small(ks[10], (N_BUCKETS, NSA_HEADS), 0.1),
        "w_out": nrm(ks[11], (DEPTH, D_MIX, D_MODEL), D_MIX, BETA),
        "ln1_g": 1.0 + small(ks[12], (DEPTH, D_MODEL), 0.02),
        "ln1_b": small(ks[13], (DEPTH, D_MODEL), 0.02),
        "w_ffn_gate": nrm(ks[14], (DEPTH, D_MODEL, D_FF), D_MODEL),
        "w_ffn_up": nrm(ks[15], (DEPTH, D_MODEL, D_FF), D_MODEL),
        "w_ffn_down": nrm(ks[16], (DEPTH, D_FF, D_MODEL), D_FF, BETA),
        "ln2_g": 1.0 + small(ks[17], (DEPTH, D_MODEL), 0.02),
        "ln2_b": small(ks[18], (DEPTH, D_MODEL), 0.02),
    }


def reference(x, w_in, hg_lb_param, hg_norm_w, cmp_pos_k, cmp_w1_k, cmp_w2_k,
              cmp_pos_v, cmp_w1_v, cmp_w2_v, rel_bias, w_out, ln1_g, ln1_b,
              w_ffn_gate, w_ffn_up, w_ffn_down, ln2_g, ln2_b):
    lbs = hgrn2_lower_bounds(hg_lb_param)
    pts = _split_points()
    for l in range(DEPTH):
        proj = x @ w_in[l]
        hq, hf, hi, hg, nq, kc, vc, ks, vs, kw, vw, gt = jnp.split(proj, pts, axis=-1)
        o_a = hgrn2_mixer(hq, hf, hi, hg, lbs[l], hg_norm_w[l])
        o_b = nsa_mixer(nq, kc, vc, ks, vs, kw, vw, gt,
                        cmp_pos_k[l], cmp_w1_k[l], cmp_w2_k[l],
                        cmp_pos_v[l], cmp_w1_v[l], cmp_w2_v[l], rel_bias)
        mix = jnp.concatenate([o_a, o_b], axis=-1) @ w_out[l]
        x = layer_norm(ALPHA * x + mix, ln1_g[l], ln1_b[l])
        ffn = (jax.nn.silu(x @ w_ffn_gate[l]) * (x @ w_ffn_up[l])) @ w_ffn_down[l]
        x = layer_norm(ALPHA * x + ffn, ln2_g[l], ln2_b[l])
    return x
```

```python
import math
from contextlib import ExitStack
import numpy as np
import concourse.bass as bass
import concourse.mybir as mybir
from concourse.bass_utils import run_bass_kernel_spmd

F32 = mybir.dt.float32
BF16 = mybir.dt.bfloat16
AF = mybir.ActivationFunctionType
ALU = mybir.AluOpType

D = 1024
SEQ = 4096
DEPTH = 4
D_IN = 3352
D_FF = 2816
ALPHA = (2.0 * DEPTH) ** 0.25
NEGM = -30000.0
N_CORES = 8


class T:
    __slots__ = ("name", "w", "r")

    def __init__(self, name):
        self.name = name
        self.w = None
        self.r = []


class Sched:
    ENG = ("pe", "act", "dve", "pool", "sp")

    def __init__(self, nc, ndma=(14, 4, 14)):
        self.nc = nc
        self.e = {"pe": nc.tensor, "act": nc.scalar, "dve": nc.vector, "pool": nc.gpsimd, "sp": nc.sync}
        self.sems = {}
        self.cnt = {}
        for k in self.ENG:
            self.sems[k] = nc.alloc_semaphore("s_" + k)
            self.cnt[k] = 0
        self.dq = {}
        for q, n in zip(("sp", "act", "pool"), ndma):
            lst = []
            for i in range(n):
                key = "d_%s_%d" % (q, i)
                self.sems[key] = nc.alloc_semaphore(key)
                self.cnt[key] = 0
                lst.append(key)
            self.dq[q] = [lst, 0]
        self.seen = {k: {} for k in self.ENG}
        self.ninst = 0
        self.nwait = 0

    def _wait(self, eng, ev):
        if ev is None:
            return
        key, val = ev
        if key == eng and eng == "pe":
            return
        if self.seen[eng].get(key, 0) >= val:
            return
        self.e[eng].wait_ge(self.sems[key], val)
        self.seen[eng][key] = val
        self.nwait += 1

    def _deps(self, eng, reads, writes):
        for t in reads:
            self._wait(eng, t.w)
        for t in writes:
            self._wait(eng, t.w)
            for ev in t.r:
                self._wait(eng, ev)

    def _commit(self, ev, reads, writes):
        for t in reads:
            t.r.append(ev)
            if len(t.r) > 16:
                best = {}
                for k, v in t.r:
                    if best.get(k, 0) < v:
                        best[k] = v
                t.r = list(best.items())
        for t in writes:
            t.w = ev
            t.r = []

    def op(self, eng, fn, reads=(), writes=()):
        self._deps(eng, reads, writes)
        ins = fn(self.e[eng])
        self.cnt[eng] += 1
        ins.then_inc(self.sems[eng], 1)
        self._commit((eng, self.cnt[eng]), reads, writes)
        self.ninst += 1
        return ins

    def dma(self, q, out, in_, reads=(), writes=(), **kw):
        lst, idx = self.dq[q]
        key = lst[idx % len(lst)]
        self.dq[q][1] = idx + 1
        if self.cnt[key] > 0:
            self._wait(q, (key, self.cnt[key]))
        self._deps(q, reads, writes)
        ins = self.e[q].dma_start(out=out, in_=in_, **kw)
        self.cnt[key] += 16
        ins.then_inc(self.sems[key], 16)
        self._commit((key, self.cnt[key]), reads, writes)
        self.ninst += 1
        return ins

    def barrier(self, engines=None):
        for eng in (engines or self.ENG):
            for key, c in self.cnt.items():
                if c > 0:
                    self._wait(eng, (key, c))


def _t5_bucket(n):
    n = np.maximum(n, 0)
    nf = np.maximum(n, 1).astype(np.float32)
    large = 16 + (np.log(nf / np.float32(16.0)) / np.float32(math.log(8.0)) * np.float32(16.0)).astype(np.int32)
    large = np.minimum(large, 31)
    return np.where(n < 16, n, large)


WS, WW, WC = 1152, 1408, 3072


def _consts(rel_bias):
    c = {}
    c["ident"] = np.eye(128, dtype=np.float32)
    s = np.arange(64)[:, None]
    t = np.arange(512)[None, :] % 64
    c["caus"] = (s <= t).astype(np.float32)
    seg = np.ones((128, SEQ), np.float32)
    seg[:, ::64] = 0.0
    c["seg"] = seg
    c["ones"] = np.ones((128, 128), np.float32)
    es = np.zeros((64, 32, 128), np.float32)
    for kt in range(32):
        es[2 * kt, kt, :64] = 1.0
        es[2 * kt + 1, kt, 64:] = 1.0
    c["esel"] = es
    tpos = np.arange(SEQ)
    cur = tpos // 64
    j = np.arange(64)[None, :]
    diff = cur[:, None] - j
    forced = (j == 0) | ((diff >= 0) & (diff < 2))
    sb = np.where(forced, 1e9, np.where(j <= cur[:, None], 0.0, -1e9)).astype(np.float32)
    c["selb"] = sb
    cs = np.arange(256) * 16
    ss = np.arange(64) * 64
    ov = np.clip(np.minimum(cs[:, None] + 32, ss[None, :] + 64) - np.maximum(cs[:, None], ss[None, :]), 0, None)
    agg = (ov / 16.0).astype(np.float32)
    agg[255] = 0.0
    c["agg"] = agg.reshape(2, 128, 64).transpose(1, 0, 2).copy()
    kl = np.arange(128)[:, None]
    col = np.arange(WW)[None, :]
    dist = col - 384 - kl
    bk = _t5_bucket(dist)
    rb = np.asarray(rel_bias, np.float32)
    c["tb"] = np.ascontiguousarray(rb[bk].transpose(2, 0, 1))
    c["mS"] = np.where(dist[:, :WS] < 0, NEGM, 0.0).astype(np.float32)
    c["mW"] = np.where((dist < 0) | (dist >= 512), NEGM, 0.0).astype(np.float32)
    colc = np.arange(WC)[None, :]
    distc = colc - 16 * kl - 31
    bkc = _t5_bucket(distc)
    c["tbc"] = np.ascontiguousarray(rb[bkc].transpose(2, 0, 1))
    c["mC"] = np.where(distc < 0, NEGM, 0.0).astype(np.float32)
    return c


CONST_SHAPES = {
    "ident": [128, 128], "caus": [64, 512], "seg": [128, SEQ], "ones": [128, 128], "esel": [64, 32, 128],
    "selb": [SEQ, 64], "agg": [128, 2, 64], "tb": [8, 128, WW], "mS": [128, WS], "mW": [128, WW],
    "tbc": [8, 128, WC], "mC": [128, WC],
}

WEIGHT_SHAPES = {
    "w_in": [DEPTH, D, D_IN], "hg_lb_param": [DEPTH, 512], "hg_norm_w": [DEPTH, 128],
    "cmp_pos_k": [DEPTH, 32, 64], "cmp_w1_k": [DEPTH, 2048, 128], "cmp_w2_k": [DEPTH, 128, 64],
    "cmp_pos_v": [DEPTH, 32, 64], "cmp_w1_v": [DEPTH, 2048, 128], "cmp_w2_v": [DEPTH, 128, 64],
    "w_out": [DEPTH, D, D], "ln1_g": [DEPTH, D], "ln1_b": [DEPTH, D],
    "w_ffn_gate": [DEPTH, D, D_FF], "w_ffn_up": [DEPTH, D, D_FF], "w_ffn_down": [DEPTH, D_FF, D],
    "ln2_g": [DEPTH, D], "ln2_b": [DEPTH, D],
}


class K:
    pass


_UID = [0]


def _sb(es, nc, name, shape, dt):
    _UID[0] += 1
    return es.enter_context(nc.sbuf_tensor("%s_u%d" % (name, _UID[0]), shape, dt))


def _ps(es, nc, name, shape, dt):
    _UID[0] += 1
    return es.enter_context(nc.psum_tensor("%s_u%d" % (name, _UID[0]), shape, dt))


def build(nseq=2, depth=DEPTH, dbg=False, stages="PHNOF", ntok=None):
    nc = bass.Bass("TRN2", target_bir_lowering=False)
    k = K()
    k.nc = nc
    k.nseq = nseq
    NT = ntok or nseq * SEQ
    k.NT = NT
    k.S = Sched(nc)
    k.x = nc.dram_tensor("x", [NT, D], F32, kind="ExternalInput").ap()
    k.out = nc.dram_tensor("out", [NT, D], F32, kind="ExternalOutput").ap()
    need = {"P": ["w_in"], "H": ["hg_norm_w"], "N": ["cmp_pos_k", "cmp_w1_k", "cmp_w2_k", "cmp_pos_v", "cmp_w1_v", "cmp_w2_v"],
            "O": ["w_out", "ln1_g", "ln1_b"], "F": ["w_ffn_gate", "w_ffn_up", "w_ffn_down", "ln2_g", "ln2_b"]}
    wn = ["hg_lb_param"] + [n for st in stages if st in need for n in need[st]]
    k.wnames = wn
    k.w = {n: nc.dram_tensor(n, [DEPTH if n == "hg_lb_param" else depth] + list(WEIGHT_SHAPES[n][1:]), F32, kind="ExternalInput").ap() for n in wn}
    k.c = {n: nc.dram_tensor("c_" + n, s, F32, kind="ExternalInput").ap() for n, s in CONST_SHAPES.items()}
    ikind = "ExternalOutput" if dbg else "Internal"
    k.fm = nc.dram_tensor("fm", [27, 128, NT], BF16, kind=ikind).ap()
    k.fmf = nc.dram_tensor("fmf", [4, 128, NT], F32, kind=ikind).ap()
    k.tmi = nc.dram_tensor("tmi", [NT, 512], BF16, kind=ikind).ap()
    k.tmv = nc.dram_tensor("tmv", [NT, 256], BF16, kind=ikind).ap()
    k.tmg = nc.dram_tensor("tmg", [NT, 24], F32, kind=ikind).ap()
    k.catT = nc.dram_tensor("catT", [8, 128, NT], BF16, kind=ikind).ap()
    k.x1 = nc.dram_tensor("x1", [NT, D], F32, kind=ikind).ap()
    k.xa = nc.dram_tensor("xa", [NT, D], F32, kind="Internal").ap()
    k.xb = nc.dram_tensor("xb", [NT, D], F32, kind="Internal").ap()
    k.lbs = nc.dram_tensor("lbs", [DEPTH, 512], F32, kind="Internal").ap()
    k.tS = nc.dram_tensor("tS", [8, 128, WS], BF16, kind="Internal").ap()
    k.tW = nc.dram_tensor("tW", [8, 128, WW], BF16, kind="Internal").ap()
    k.tC = nc.dram_tensor("tC", [8, 128, WC], BF16, kind="Internal").ap()

    if "X" not in stages:
        stage_setup(k)
        k.S.barrier()
    xin = k.x
    for l in range(depth):
        last = l == depth - 1
        xout = k.out if last else (k.xa if l % 2 == 0 else k.xb)
        if "P" in stages:
            stage_proj(k, l, xin)
            k.S.barrier()
        if "H" in stages:
            stage_hgrn(k, l)
            k.S.barrier()
        if "N" in stages:
            stage_nsa(k, l)
            k.S.barrier()
        if "O" in stages:
            stage_out(k, l, xin)
            k.S.barrier()
        if "F" in stages:
            stage_ffn(k, l, xout)
            k.S.barrier()
        xin = xout
    k.S.barrier(["sp"])
    return k


def stage_setup(k):
    nc, S = k.nc, k.S
    with ExitStack() as es:
        lp = _sb(es, nc, "su_lp", [128, DEPTH, 4], F32)
        tl = T("lp")
        S.dma("sp", lp[:], k.w["hg_lb_param"].rearrange("l (q p) -> p l q", p=128), writes=[tl],
              allow_slow_non_contiguous=True)
        ex = _sb(es, nc, "su_ex", [128, DEPTH, 4], F32)
        te = T("ex")
        S.op("act", lambda e: e.activation(out=ex[:], in_=lp[:], func=AF.Exp), reads=[tl], writes=[te])
        sm = _sb(es, nc, "su_sm", [128, 4], F32)
        ts = T("sm")
        S.op("dve", lambda e: e.tensor_tensor(out=sm[:], in0=ex[:, 0, :], in1=ex[:, 1, :], op=ALU.add), reads=[te], writes=[ts])
        for l in range(2, DEPTH):
            S.op("dve", lambda e, l=l: e.tensor_tensor(out=sm[:], in0=sm[:], in1=ex[:, l, :], op=ALU.add), reads=[te, ts], writes=[ts])
        S.op("dve", lambda e: e.reciprocal(out=sm[:], in_=sm[:]), reads=[ts], writes=[ts])
        lb = _sb(es, nc, "su_lb", [128, DEPTH, 4], F32)
        tb_ = T("lb")
        S.op("dve", lambda e: e.memset(lb[:], 0.0), writes=[tb_])
        for l in range(1, DEPTH):
            S.op("dve", lambda e, l=l: e.tensor_tensor(out=ex[:, l, :], in0=ex[:, l, :], in1=sm[:], op=ALU.mult), reads=[te, ts], writes=[te])
            S.op("dve", lambda e, l=l: e.tensor_tensor(out=lb[:, l, :], in0=lb[:, l - 1, :], in1=ex[:, l, :], op=ALU.add), reads=[te, tb_], writes=[tb_])
        S.dma("sp", k.lbs.rearrange("l (q p) -> p l q", p=128), lb[:], reads=[tb_], allow_slow_non_contiguous=True)
        S.barrier()
    for (src, msk, dst, W) in (("tb", "mS", k.tS, WS), ("tb", "mW", k.tW, WW), ("tbc", "mC", k.tC, WC)):
        with ExitStack() as es:
            mt = _sb(es, nc, "su_m_" + msk, [128, W], F32)
            tm = T("m")
            S.dma("sp", mt[:], k.c[msk], writes=[tm])
            bt = [_sb(es, nc, "su_b_%s_%d" % (msk, i), [128, W], F32) for i in range(2)]
            ot = [_sb(es, nc, "su_o_%s_%d" % (msk, i), [128, W], BF16) for i in range(2)]
            tbt = [T("b0"), T("b1")]
            tot = [T("o0"), T("o1")]
            for h in range(8):
                i = h % 2
                S.dma("sp", bt[i][:], k.c[src][h, :, 0:W], writes=[tbt[i]])
                S.op("dve", lambda e: e.tensor_tensor(out=ot[i][:], in0=bt[i][:], in1=mt[:], op=ALU.add), reads=[tbt[i], tm], writes=[tot[i]])
                S.dma("sp", dst[h], ot[i][:], reads=[tot[i]])
            S.barrier()


PSKIP = ''
NSKIP = ''
FM_GROUPS = [[0, 1, 2, 3], [12, 13, 14, 15], [16, 17, 18, 19], [20, 21, 22], [24]]


def load_consts_bf(k, es, names):
    nc, S = k.nc, k.S
    out = {}
    for n in names:
        shp = CONST_SHAPES[n]
        t = _sb(es, nc, "cb_" + n, shp, BF16)
        tt = T("cb_" + n)
        S.dma("pool", t[:], k.c[n], writes=[tt])
        out[n] = (t, tt)
    return out


def transpose_block(k, xsrc, txsrc, xT, txT, ident, tid, psT, tpsT, ncols, ctr):
    S = k.S
    nj = ncols // 128
    for kc in range(8):
        pi = ctr[0] % len(psT)
        ctr[0] += 1
        for j in range(nj):
            S.op("pe", lambda e, j=j, kc=kc, pi=pi: e.transpose(out=psT[pi][:, j * 128:(j + 1) * 128], in_=xsrc[:, j, kc * 128:(kc + 1) * 128], identity=ident),
                 reads=[txsrc, tid], writes=[tpsT[pi]])
        eng = "act" if kc % 2 == 0 else "dve"
        if eng == "act":
            S.op("act", lambda e, kc=kc, pi=pi: e.copy(out=xT[:, kc, :], in_=psT[pi][:, 0:ncols]), reads=[tpsT[pi]], writes=[txT[kc]])
        else:
            S.op("dve", lambda e, kc=kc, pi=pi: e.tensor_copy(out=xT[:, kc, :], in_=psT[pi][:, 0:ncols]), reads=[tpsT[pi]], writes=[txT[kc]])


def stage_proj(k, l, xin):
    nc, S = k.nc, k.S
    NT = k.NT
    nblk = NT // 512
    with ExitStack() as es:
        cb = load_consts_bf(k, es, ["ident"])
        ident, tid = cb["ident"]
        wsb = _sb(es, nc, "p_w", [128, 8, D_IN], BF16)
        tw = [T("w%d" % i) for i in range(8)]
        for kc in range(8):
            S.dma("pool", wsb[:, kc, :], k.w["w_in"][l, kc * 128:(kc + 1) * 128, :], writes=[tw[kc]])
        xt = [_sb(es, nc, "p_xt%d" % i, [128, 4, D], F32) for i in range(2)]
        txt = [T("xt%d" % i) for i in range(2)]
        xb = _sb(es, nc, "p_xb", [128, 4, D], BF16)
        txb = T("xb")
        xT = [_sb(es, nc, "p_xT%d" % i, [128, 8, 512], BF16) for i in range(2)]
        txT = [[T("xT%d_%d" % (i, kc)) for kc in range(8)] for i in range(2)]
        stg = [_sb(es, nc, "p_stg%d" % i, [128, 27, 512], BF16) for i in range(2)]
        tstg = [[T("stg%d_%d" % (i, g)) for g in range(len(FM_GROUPS))] for i in range(2)]
        stf = [_sb(es, nc, "p_stf%d" % i, [128, 4, 512], F32) for i in range(2)]
        tstf = [T("stf%d" % i) for i in range(2)]
        sti = [_sb(es, nc, "p_sti%d" % i, [128, 4, 512], BF16) for i in range(2)]
        tsti = [T("sti%d" % i) for i in range(2)]
        stv = [_sb(es, nc, "p_stv%d" % i, [128, 4, 256], BF16) for i in range(2)]
        tstv = [T("stv%d" % i) for i in range(2)]
        stgt = [_sb(es, nc, "p_stgt%d" % i, [128, 4, 24], F32) for i in range(2)]
        tstgt = [T("stgt%d" % i) for i in range(2)]
        psT = [_ps(es, nc, "p_psT%d" % i, [128, 1024], BF16) for i in range(2)]
        tpsT = [T("psT%d" % i) for i in range(2)]
        psm = [_ps(es, nc, "p_psm%d" % i, [128, 512], F32) for i in range(5)]
        tpsm = [T("psm%d" % i) for i in range(5)]
        ctr = [0]
        pctr = [0]
        ectr = [0]

        def evac(dst, src, reads, writes, scale=None, eng=None):
            if eng is None:
                eng = "act" if ectr[0] % 2 == 0 else "dve"
                ectr[0] += 1
            if eng == "act":
                if scale is None:
                    S.op("act", lambda e: e.copy(out=dst, in_=src), reads=reads, writes=writes)
                else:
                    S.op("act", lambda e: e.mul(out=dst, in_=src, mul=scale), reads=reads, writes=writes)
            else:
                if scale is None:
                    S.op("dve", lambda e: e.tensor_copy(out=dst, in_=src), reads=reads, writes=writes)
                else:
                    S.op("dve", lambda e: e.tensor_scalar(out=dst, in0=src, scalar1=scale, scalar2=None, op0=ALU.mult), reads=reads, writes=writes)

        def load_x(b):
            S.dma("sp", xt[b % 2][:], xin[b * 512:(b + 1) * 512, :].rearrange("(j p) d -> p j d", p=128), writes=[txt[b % 2]])

        load_x(0)
        for b in range(nblk):
            bi = b % 2
            if b + 1 < nblk:
                load_x(b + 1)
            S.op("dve", lambda e: e.tensor_copy(out=xb[:], in_=xt[bi][:]), reads=[txt[bi]], writes=[txb])
            transpose_block(k, xb, txb, xT[bi], txT[bi], ident[:], tid, psT, tpsT, 512, ctr)
            cols = slice(b * 512, (b + 1) * 512)
            for gi, grp in enumerate([] if 'f' in PSKIP else FM_GROUPS):
                for m in grp:
                    pi = pctr[0] % 5
                    pctr[0] += 1
                    for kc in range(8):
                        S.op("pe", lambda e, m=m, kc=kc, pi=pi: e.matmul(psm[pi][:], lhsT=wsb[:, kc, m * 128:(m + 1) * 128], rhs=xT[bi][:, kc, :], start=(kc == 0), stop=(kc == 7)),
                             reads=[tw[kc], txT[bi][kc]], writes=[tpsm[pi]])
                    evac(stg[bi][:, m, :], psm[pi][:], [tpsm[pi]], [tstg[bi][gi]], scale=(0.125 if 16 <= m < 20 else None))
                S.dma("sp", k.fm[grp[0]:grp[-1] + 1, :, cols].rearrange("c p t -> p c t"), stg[bi][:, grp[0]:grp[-1] + 1, :], reads=[tstg[bi][gi]])
            for m in ([] if 'h' in PSKIP else range(4, 8)):
                pi = pctr[0] % 5
                pctr[0] += 1
                for kc in range(8):
                    S.op("pe", lambda e, m=m, kc=kc, pi=pi: e.matmul(psm[pi][:], lhsT=wsb[:, kc, m * 128:(m + 1) * 128], rhs=xT[bi][:, kc, :], start=(kc == 0), stop=(kc == 7)),
                         reads=[tw[kc], txT[bi][kc]], writes=[tpsm[pi]])
                evac(stf[bi][:, m - 4, :], psm[pi][:], [tpsm[pi]], [tstf[bi]])
            if 'h' not in PSKIP:
                S.dma("sp", k.fmf[:, :, cols].rearrange("c p t -> p c t"), stf[bi][:], reads=[tstf[bi]])
            for j in ([] if 'i' in PSKIP else range(4)):
                pi = pctr[0] % 5
                pctr[0] += 1
                for kc in range(8):
                    S.op("pe", lambda e, j=j, kc=kc, pi=pi: e.matmul(psm[pi][:], lhsT=xT[bi][:, kc, j * 128:(j + 1) * 128], rhs=wsb[:, kc, 1024:1536], start=(kc == 0), stop=(kc == 7)),
                         reads=[tw[kc], txT[bi][kc]], writes=[tpsm[pi]])
                evac(sti[bi][:, j, :], psm[pi][:], [tpsm[pi]], [tsti[bi]])
            if 'i' not in PSKIP:
                S.dma("pool", k.tmi[b * 512:(b + 1) * 512, :].rearrange("(j p) c -> p j c", p=128), sti[bi][:], reads=[tsti[bi]])
            for j in ([] if 'v' in PSKIP else range(4)):
                pi = pctr[0] % 5
                pctr[0] += 1
                for (c0, c1, o0) in ((2944, 3072, 0), (3200, 3328, 128), (3328, 3352, 256)):
                    if 'g' in PSKIP and o0 == 256:
                        continue
                    for kc in range(8):
                        S.op("pe", lambda e, j=j, kc=kc, pi=pi, c0=c0, c1=c1, o0=o0: e.matmul(psm[pi][:, o0:o0 + (c1 - c0)], lhsT=xT[bi][:, kc, j * 128:(j + 1) * 128], rhs=wsb[:, kc, c0:c1], start=(kc == 0), stop=(kc == 7)),
                             reads=[tw[kc], txT[bi][kc]], writes=[tpsm[pi]])
                evac(stv[bi][:, j, :], psm[pi][:, 0:256], [tpsm[pi]], [tstv[bi]], eng="dve")
                if 'G' not in PSKIP:
                    evac(stgt[bi][:, j, :], psm[pi][:, 256:280], [tpsm[pi]], [tstgt[bi]], eng="dve")
            if 'v' not in PSKIP:
              S.dma("pool", k.tmv[b * 512:(b + 1) * 512, :].rearrange("(j p) c -> p j c", p=128), stv[bi][:], reads=[tstv[bi]])
              if 'D' not in PSKIP:
                S.dma("sp" if 'Q' in PSKIP else "pool", k.tmg[b * 512:(b + 1) * 512, :].rearrange("(j p) c -> p j c", p=128), stgt[bi][:], reads=[tstgt[bi]])
        S.barrier()


def stage_hgrn(k, l):
    nc, S = k.nc, k.S
    with ExitStack() as es:
        cb = load_consts_bf(k, es, ["ident", "caus", "seg", "ones"])
        ident, tid = cb["ident"]
        caus, tcaus = cb["caus"]
        seg, tseg = cb["seg"]
        ones, tones = cb["ones"]
        lbt = _sb(es, nc, "h_lb", [128, 4], F32)
        tlb = T("lb")
        S.dma("sp", lbt[:], k.lbs[l].rearrange("(q p) -> p q", p=128), writes=[tlb], allow_slow_non_contiguous=True)
        oml = _sb(es, nc, "h_oml", [128, 4], F32)
        toml = T("oml")
        S.op("dve", lambda e: e.tensor_scalar(out=oml[:], in0=lbt[:], scalar1=-1.0, scalar2=1.0, op0=ALU.mult, op1=ALU.add), reads=[tlb], writes=[toml])
        gn = _sb(es, nc, "h_gn", [128, 1], F32)
        tgn = T("gn")
        S.dma("sp", gn[:], k.w["hg_norm_w"][l].rearrange("(p o) -> p o", o=1), writes=[tgn], allow_slow_non_contiguous=True)

        fT = _sb(es, nc, "h_f", [128, SEQ], F32); tf = T("f")
        qT = _sb(es, nc, "h_q", [128, SEQ], BF16); tq = T("q")
        gT = _sb(es, nc, "h_g", [128, SEQ], BF16); tg = T("g")
        vS = _sb(es, nc, "h_v", [64, 64, 128], BF16); tv = T("v")
        A = _sb(es, nc, "h_A", [128, SEQ], F32); tA = T("A")
        B = _sb(es, nc, "h_B", [128, SEQ], F32); tB = T("B")
        C = _sb(es, nc, "h_C", [128, SEQ], F32); tC = T("C")
        Dd = _sb(es, nc, "h_D", [128, SEQ], F32); tD = T("D")
        qt = _sb(es, nc, "h_qt", [128, SEQ], BF16); tqt = T("qt")
        kt_ = _sb(es, nc, "h_kt", [128, SEQ], BF16); tkt = T("kt")
        qb = _sb(es, nc, "h_qb", [128, SEQ], BF16); tqb = T("qb")
        kd = _sb(es, nc, "h_kd", [128, SEQ], BF16); tkd = T("kd")
        ebl = _sb(es, nc, "h_ebl", [128, 64], F32); tebl = T("ebl")
        AT = _sb(es, nc, "h_AT", [64, SEQ], BF16); tAT = [T("AT%d" % i) for i in range(8)]
        kdT = _sb(es, nc, "h_kdT", [64, 64, 128], BF16); tkdT = [T("kdT%d" % i) for i in range(8)]
        oT = _sb(es, nc, "h_oT", [128, SEQ], F32); toT = [T("oT%d" % i) for i in range(8)]
        St = _sb(es, nc, "h_S", [128, 128], F32); tSt = T("S")
        Sb = _sb(es, nc, "h_Sb", [128, 128], BF16); tSb = T("Sb")
        ostg = _sb(es, nc, "h_ostg", [128, SEQ], BF16); tostg = T("ostg")
        rstd = _sb(es, nc, "h_rstd", [128, 512], F32); trstd = T("rstd")
        psA = [_ps(es, nc, "h_psA%d" % i, [128, 512], F32) for i in range(2)]; tpsA = [T("psA%d" % i) for i in range(2)]
        psK = [_ps(es, nc, "h_psK%d" % i, [64, 1024], BF16) for i in range(2)]; tpsK = [T("psK%d" % i) for i in range(2)]
        psO = [_ps(es, nc, "h_psO%d" % i, [128, 512], F32) for i in range(2)]; tpsO = [T("psO%d" % i) for i in range(2)]
        psS = [_ps(es, nc, "h_psS%d" % i, [128, 512], F32) for i in range(2)]; tpsS = [T("psS%d" % i) for i in range(2)]

        def v3(t):
            return t[:].rearrange("p (c j) -> p c j", j=64)

        for s in range(k.nseq):
            for h in range(4):
                cols = slice(s * SEQ, (s + 1) * SEQ)
                S.dma("sp", fT[:], k.fmf[h, :, cols], writes=[tf])
                S.dma("sp", qT[:], k.fm[h, :, cols], writes=[tq])
                S.dma("sp", gT[:], k.fm[12 + h, :, cols], writes=[tg])
                S.dma("pool", vS[:], k.tmi[s * SEQ:(s + 1) * SEQ, h * 128:(h + 1) * 128].rearrange("(c j) v -> j c v", j=64), writes=[tv])
                S.op("act", lambda e: e.activation(out=A[:], in_=fT[:], func=AF.Sigmoid), reads=[tf], writes=[tA])
                S.op("dve", lambda e: e.tensor_scalar(out=A[:], in0=A[:], scalar1=oml[:, h:h + 1], scalar2=lbt[:, h:h + 1], op0=ALU.mult, op1=ALU.add), reads=[tA, toml, tlb], writes=[tA])
                S.op("act", lambda e: e.activation(out=B[:], in_=A[:], func=AF.Ln), reads=[tA], writes=[tB])
                S.op("dve", lambda e: e.tensor_tensor_scan(out=C[:], data0=seg[:], data1=B[:], initial=0.0, op0=ALU.mult, op1=ALU.add), reads=[tB, tseg], writes=[tC])
                S.op("dve", lambda e: e.tensor_scalar(out=A[:], in0=A[:], scalar1=-1.0, scalar2=1.0, op0=ALU.mult, op1=ALU.add), reads=[tA], writes=[tA])
                C3 = v3(C)
                S.op("act", lambda e: e.activation(out=B[:], in_=C[:], func=AF.Exp), reads=[tC], writes=[tB])
                S.op("dve", lambda e: e.tensor_tensor(out=qb[:], in0=B[:], in1=qT[:], op=ALU.mult), reads=[tB, tq], writes=[tqb])
                S.op("act", lambda e: e.activation(out=ebl[:], in_=C3[:, :, 63], func=AF.Exp), reads=[tC], writes=[tebl])
                S.op("dve", lambda e: e.tensor_tensor(out=v3(Dd), in0=C3, in1=C3[:, :, 31:32].to_broadcast([128, 64, 64]), op=ALU.subtract), reads=[tC], writes=[tD])
                S.op("act", lambda e: e.activation(out=B[:], in_=Dd[:], func=AF.Exp), reads=[tD], writes=[tB])
                S.op("dve", lambda e: e.tensor_tensor(out=qt[:], in0=B[:], in1=qT[:], op=ALU.mult), reads=[tB, tq], writes=[tqt])
                S.op("act", lambda e: e.activation(out=B[:], in_=Dd[:], func=AF.Exp, scale=-1.0), reads=[tD], writes=[tB])
                S.op("dve", lambda e: e.tensor_tensor(out=kt_[:], in0=B[:], in1=A[:], op=ALU.mult), reads=[tB, tA], writes=[tkt])
                S.op("dve", lambda e: e.tensor_tensor(out=v3(Dd), in0=C3[:, :, 63:64].to_broadcast([128, 64, 64]), in1=C3, op=ALU.subtract), reads=[tC], writes=[tD])
                S.op("act", lambda e: e.activation(out=B[:], in_=Dd[:], func=AF.Exp), reads=[tD], writes=[tB])
                S.op("dve", lambda e: e.tensor_tensor(out=kd[:], in0=B[:], in1=A[:], op=ALU.mult), reads=[tB, tA], writes=[tkd])
                for blk in range(8):
                    pi = blk % 2
                    for cc in range(8):
                        c = blk * 8 + cc
                        S.op("pe", lambda e, c=c, cc=cc, pi=pi: e.matmul(psA[pi][0:64, cc * 64:(cc + 1) * 64], lhsT=kt_[:, c * 64:(c + 1) * 64], rhs=qt[:, c * 64:(c + 1) * 64], start=True, stop=True),
                             reads=[tkt, tqt], writes=[tpsA[pi]])
                    S.op("dve", lambda e, blk=blk, pi=pi: e.tensor_tensor(out=AT[:, blk * 512:(blk + 1) * 512], in0=psA[pi][0:64, :], in1=caus[:], op=ALU.mult),
                         reads=[tpsA[pi], tcaus], writes=[tAT[blk]])
                    for cc in range(8):
                        c = blk * 8 + cc
                        S.op("pe", lambda e, c=c, cc=cc, pi=pi: e.transpose(out=psK[pi][:, cc * 128:(cc + 1) * 128], in_=kd[:, c * 64:(c + 1) * 64], identity=ident[:]),
                             reads=[tkd, tid], writes=[tpsK[pi]])
                    S.op("act", lambda e, blk=blk, pi=pi: e.copy(out=kdT[:, blk * 8:(blk + 1) * 8, :], in_=psK[pi][:].rearrange("p (c d) -> p c d", d=128)),
                         reads=[tpsK[pi]], writes=[tkdT[blk]])
                for c in range(64):
                    blk, cc = c // 8, c % 8
                    pi = blk % 2
                    S.op("pe", lambda e, c=c, cc=cc, pi=pi: e.matmul(psO[pi][:, cc * 64:(cc + 1) * 64], lhsT=vS[:, c, :], rhs=AT[:, c * 64:(c + 1) * 64], start=True, stop=(c == 0)),
                         reads=[tv, tAT[blk]], writes=[tpsO[pi]])
                    if c > 0:
                        S.op("pe", lambda e, c=c, cc=cc, pi=pi: e.matmul(psO[pi][:, cc * 64:(cc + 1) * 64], lhsT=Sb[:], rhs=qb[:, c * 64:(c + 1) * 64], start=False, stop=True),
                             reads=[tSb, tqb], writes=[tpsO[pi]])
                    if cc == 7:
                        S.op("act", lambda e, blk=blk, pi=pi: e.copy(out=oT[:, blk * 512:(blk + 1) * 512], in_=psO[pi][:]), reads=[tpsO[pi]], writes=[toT[blk]])
                    if c < 63:
                        si = c % 2
                        S.op("pe", lambda e, c=c, si=si: e.matmul(psS[si][:, 0:128], lhsT=kdT[:, c, :], rhs=vS[:, c, :], start=True, stop=True),
                             reads=[tkdT[blk], tv], writes=[tpsS[si]])
                        if c == 0:
                            S.op("dve", lambda e, si=si: e.tensor_copy(out=St[:], in_=psS[si][:, 0:128]), reads=[tpsS[si]], writes=[tSt])
                        else:
                            S.op("dve", lambda e, c=c, si=si: e.scalar_tensor_tensor(out=St[:], in0=St[:], scalar=ebl[:, c:c + 1], in1=psS[si][:, 0:128], op0=ALU.mult, op1=ALU.add),
                                 reads=[tSt, tebl, tpsS[si]], writes=[tSt])
                        S.op("act", lambda e: e.copy(out=Sb[:], in_=St[:]), reads=[tSt], writes=[tSb])
                S.op("act", lambda e: e.activation(out=qt[:], in_=oT[:], func=AF.Square), reads=toT, writes=[tqt])
                S.op("act", lambda e: e.activation(out=kd[:], in_=gT[:], func=AF.Silu), reads=[tg], writes=[tkd])
                for blk in range(8):
                    pi = blk % 2
                    bs = slice(blk * 512, (blk + 1) * 512)
                    S.op("pe", lambda e, bs=bs, pi=pi: e.matmul(psA[pi][:], lhsT=ones[:], rhs=qt[:, bs], start=True, stop=True), reads=[tones, tqt], writes=[tpsA[pi]])
                    S.op("dve", lambda e, pi=pi: e.tensor_scalar(out=rstd[:], in0=psA[pi][:], scalar1=1.0 / 128.0, scalar2=1e-6, op0=ALU.mult, op1=ALU.add), reads=[tpsA[pi]], writes=[trstd])
                    S.op("act", lambda e: e.activation(out=rstd[:], in_=rstd[:], func=AF.Sqrt), reads=[trstd], writes=[trstd])
                    S.op("dve", lambda e: e.reciprocal(out=rstd[:], in_=rstd[:]), reads=[trstd], writes=[trstd])
                    S.op("dve", lambda e, bs=bs: e.tensor_tensor(out=rstd[:], in0=rstd[:], in1=oT[:, bs], op=ALU.mult), reads=[trstd, toT[blk]], writes=[trstd])
                    S.op("dve", lambda e, bs=bs: e.scalar_tensor_tensor(out=ostg[:, bs], in0=rstd[:], scalar=gn[:, 0:1], in1=kd[:, bs], op0=ALU.mult, op1=ALU.mult),
                         reads=[trstd, tgn, tkd], writes=[tostg])
                S.dma("sp", k.catT[h, :, cols], ostg[:], reads=[tostg])
        S.barrier()


def stage_nsa(k, l):
    nc, S = k.nc, k.S
    with ExitStack() as es:
        cb = load_consts_bf(k, es, ["ident", "esel", "agg"])
        ident, tid = cb["ident"]
        esel, tesel = cb["esel"]
        aggb, tagg = cb["agg"]
        idf = _sb(es, nc, "n_idf", [128, 128], F32); tidf = T("idf")
        S.dma("sp", idf[:], k.c["ident"], writes=[tidf])
        selb = _sb(es, nc, "n_selb", [128, 32, 64], F32); tselb = T("selb")
        S.dma("sp", selb[:], k.c["selb"].rearrange("(c p) j -> p c j", p=128), writes=[tselb])
        w1 = {}
        w2 = {}
        pos = {}
        hb = {}
        for kv in "kv":
            w1[kv] = (_sb(es, nc, "n_w1" + kv, [128, 32, 128], BF16), T("w1" + kv))
            for half in range(2):
                S.dma("pool", w1[kv][0][half * 64:(half + 1) * 64, :, :], k.w["cmp_w1_" + kv][l].rearrange("(p d) h -> d p h", d=64), writes=[w1[kv][1]])
            pos[kv] = (_sb(es, nc, "n_pos" + kv, [64, 32], BF16), T("pos" + kv))
            S.dma("pool", pos[kv][0][:], k.w["cmp_pos_" + kv][l].rearrange("p d -> d p"), writes=[pos[kv][1]], allow_slow_non_contiguous=True)
            hb[kv] = (_sb(es, nc, "n_hb" + kv, [128, 1], F32), T("hb" + kv))
        w2[("k")] = (_sb(es, nc, "n_w2k", [128, 128], BF16), T("w2k"))
        for half in range(2):
            S.dma("pool", w2["k"][0][:, half * 64:(half + 1) * 64], k.w["cmp_w2_k"][l], writes=[w2["k"][1]])
        w2["v"] = (_sb(es, nc, "n_w2v", [128, 64], BF16), T("w2v"))
        S.dma("pool", w2["v"][0][:], k.w["cmp_w2_v"][l], writes=[w2["v"][1]])

        qT = [_sb(es, nc, "n_q%d" % i, [128, SEQ], BF16) for i in range(2)]; tq = [T("q%d" % i) for i in range(2)]
        ksT = _sb(es, nc, "n_ks", [128, SEQ], BF16); tks = T("ks")
        kwT = _sb(es, nc, "n_kw", [128, SEQ], BF16); tkw = T("kw")
        srcT = {kv: (_sb(es, nc, "n_src" + kv, [128, SEQ], BF16), T("src" + kv)) for kv in "kv"}
        vs = _sb(es, nc, "n_vs", [128, 32, 128], BF16); tvs = T("vs")
        vw = _sb(es, nc, "n_vw", [128, 32, 128], BF16); tvw = T("vw")
        kcT = _sb(es, nc, "n_kcT", [128, 256], BF16); tkcT = T("kcT")
        vca = _sb(es, nc, "n_vca", [128, 2, 128], BF16); tvca = T("vca")
        gt = _sb(es, nc, "n_gt", [128, 32, 24], F32); tgt = T("gt")
        tabS = _sb(es, nc, "n_tS", [128, 4, WS], BF16); ttS = T("tS")
        tabW = _sb(es, nc, "n_tW", [128, 4, WW], BF16); ttW = T("tW")
        tabC = _sb(es, nc, "n_tC", [128, 4, WC], BF16); ttC = T("tC")
        gu = _sb(es, nc, "n_gu", [128, 256], F32); tgu = T("gu")
        gu2 = _sb(es, nc, "n_gu2", [128, 256], F32); tgu2 = T("gu2")
        ge = _sb(es, nc, "n_ge", [128, 256], BF16); tge = T("ge")
        nselT = _sb(es, nc, "n_nselT", [64, 512], BF16); tnselT = T("nselT")
        Eb = [_sb(es, nc, "n_E%d" % i, [128, 512], BF16) for i in range(3)]; tE = [T("E%d" % i) for i in range(3)]
        oacc = [_sb(es, nc, "n_oacc%d" % i, [128, 4, 4, 64], F32) for i in range(2)]; toacc = [T("oacc0"), T("oacc1")]
        imp = _sb(es, nc, "n_imp", [128, 4, 64], F32); timp = T("imp")
        rc4 = _sb(es, nc, "n_rc4", [128, 4], F32); trc4 = T("rc4")
        cf4 = _sb(es, nc, "n_cf4", [128, 4], F32); tcf4 = T("cf4")
        tmp4 = _sb(es, nc, "n_tmp4", [128, 4, 64], F32); ttmp4 = T("tmp4")
        sc = _sb(es, nc, "n_sc", [128, 64], F32); tsc = T("sc")
        sc2 = _sb(es, nc, "n_sc2", [128, 64], F32); tsc2 = T("sc2")
        m8 = _sb(es, nc, "n_m8", [128, 8], F32); tm8 = T("m8")
        nsel = _sb(es, nc, "n_nsel", [128, 4, 64], F32); tnsel = T("nsel")
        ostg = _sb(es, nc, "n_ostg", [128, 2, 512], BF16); tostg = T("ostg")

        psS = [_ps(es, nc, "n_psS%d" % i, [128, 512], F32) for i in range(2)]; tpsS = [T("psS%d" % i) for i in range(2)]
        psCA = _ps(es, nc, "n_psCA", [128, 512], F32); tpsCA = T("psCA")
        psCB = _ps(es, nc, "n_psCB", [128, 512], F32); tpsCB = T("psCB")
        psOS = _ps(es, nc, "n_psOS", [128, 512], F32); tpsOS = T("psOS")
        psOW = _ps(es, nc, "n_psOW", [128, 512], F32); tpsOW = T("psOW")
        psT = _ps(es, nc, "n_psT", [128, 512], F32); tpsT = T("psT")
        psX = _ps(es, nc, "n_psX", [128, 512], F32); tpsX = T("psX")
        sctr = [0]
        ectr = [0]

        S.op("dve", lambda e: e.memset(vs[:], 1.0), writes=[tvs])
        S.op("dve", lambda e: e.memset(vw[:], 1.0), writes=[tvw])

        def score_tile(kT_ap, kreads, q_ap, qreads, tab_ap, tabreads, extra=None, cr=(0, 512)):
            c0, c1 = cr
            pi = sctr[0] % 2
            sctr[0] += 1
            S.op("pe", lambda e: e.matmul(psS[pi][:, c0:c1], lhsT=kT_ap, rhs=q_ap[:, c0:c1], start=True, stop=False), reads=kreads + qreads, writes=[tpsS[pi]])
            S.op("pe", lambda e: e.matmul(psS[pi][:, c0:c1], lhsT=ident[:], rhs=tab_ap[:, c0:c1], start=False, stop=(extra is None)), reads=[tid] + tabreads, writes=[tpsS[pi]])
            if extra is not None:
                S.op("pe", lambda e: e.matmul(psS[pi][:, c0:c1], lhsT=extra[0], rhs=extra[1][:, c0:c1], start=False, stop=True), reads=extra[2], writes=[tpsS[pi]])
            ei = ectr[0] % 3
            ectr[0] += 1
            S.op("act", lambda e: e.activation(out=Eb[ei][:, c0:c1], in_=psS[pi][:, c0:c1], func=AF.Exp), reads=[tpsS[pi]], writes=[tE[ei]])
            return ei

        for s in range(k.nseq):
            cols = slice(s * SEQ, (s + 1) * SEQ)
            S.dma("sp", gt[:], k.tmg[s * SEQ:(s + 1) * SEQ, :].rearrange("(c p) g -> p c g", p=128), writes=[tgt])
            S.op("act", lambda e: e.activation(out=gt[:], in_=gt[:], func=AF.Sigmoid), reads=[tgt], writes=[tgt])
            for g in range(2):
                for i in range(2):
                    S.dma("sp", qT[i][:], k.fm[16 + 2 * g + i, :, cols], writes=[tq[i]])
                for half in range(2):
                    S.dma("sp", ksT[half * 64:(half + 1) * 64, :], k.fm[22, g * 64:(g + 1) * 64, cols], writes=[tks])
                    S.dma("sp", kwT[half * 64:(half + 1) * 64, :], k.fm[24, g * 64:(g + 1) * 64, cols], writes=[tkw])
                S.dma("sp", srcT["k"][0][0:64, :], k.fm[20, g * 64:(g + 1) * 64, cols], writes=[srcT["k"][1]])
                S.dma("sp", srcT["v"][0][0:64, :], k.fm[21, g * 64:(g + 1) * 64, cols], writes=[srcT["v"][1]])
                S.dma("pool", vs[:, :, 0:64], k.tmv[s * SEQ:(s + 1) * SEQ, g * 64:(g + 1) * 64].rearrange("(c p) d -> p c d", p=128), writes=[tvs])
                S.dma("pool", vw[:, :, 0:64], k.tmv[s * SEQ:(s + 1) * SEQ, 128 + g * 64:128 + (g + 1) * 64].rearrange("(c p) d -> p c d", p=128), writes=[tvw])
                S.dma("pool", tabS[:], k.tS[4 * g:4 * g + 4].rearrange("h p w -> p h w"), writes=[ttS])
                S.dma("pool", tabW[:], k.tW[4 * g:4 * g + 4].rearrange("h p w -> p h w"), writes=[ttW])
                S.dma("pool", tabC[:], k.tC[4 * g:4 * g + 4].rearrange("h p w -> p h w"), writes=[ttC])
                for kv in ("" if 'c' in NSKIP else "kv"):
                    src, tsrc = srcT[kv]
                    src3 = src[0:64, :].rearrange("q (n s) -> q n s", s=16)
                    w1t, tw1 = w1[kv]
                    for p in range(32):
                        S.op("pe", lambda e, p=p: e.matmul(psX[:, 300:301], lhsT=w1t[0:64, p, :], rhs=pos[kv][0][:, p:p + 1], start=(p == 0), stop=(p == 31)),
                             reads=[tw1, pos[kv][1]], writes=[tpsX])
                    S.op("dve", lambda e: e.tensor_copy(out=hb[kv][0][:], in_=psX[:, 300:301]), reads=[tpsX], writes=[hb[kv][1]])
                    for p in range(32):
                        S.op("pe", lambda e, p=p: e.matmul(psX[:, 0:255], lhsT=w1t[0:64, p, :], rhs=src3[:, (p // 16):(p // 16) + 255, p % 16], start=(p == 0), stop=(p == 31)),
                             reads=[tw1, tsrc], writes=[tpsX])
                    S.op("dve", lambda e: e.memset(ge[:], 0.0), writes=[tge])
                    S.op("act", lambda e: e.activation(out=gu[:, 0:255], in_=psX[:, 0:255], func=AF.Identity, bias=hb[kv][0][:, 0:1]), reads=[tpsX, hb[kv][1]], writes=[tgu])
                    S.op("dve", lambda e: e.tensor_tensor(out=gu2[:, 0:255], in0=gu[:, 0:255], in1=gu[:, 0:255], op=ALU.mult), reads=[tgu], writes=[tgu2])
                    S.op("dve", lambda e: e.tensor_scalar(out=gu2[:, 0:255], in0=gu2[:, 0:255], scalar1=0.044715, scalar2=1.0, op0=ALU.mult, op1=ALU.add), reads=[tgu2], writes=[tgu2])
                    S.op("dve", lambda e: e.tensor_tensor(out=gu2[:, 0:255], in0=gu2[:, 0:255], in1=gu[:, 0:255], op=ALU.mult), reads=[tgu2, tgu], writes=[tgu2])
                    S.op("act", lambda e: e.activation(out=gu2[:, 0:255], in_=gu2[:, 0:255], func=AF.Sigmoid, scale=2.0 * 0.7978845608028654), reads=[tgu2], writes=[tgu2])
                    S.op("dve", lambda e: e.tensor_tensor(out=ge[:, 0:255], in0=gu2[:, 0:255], in1=gu[:, 0:255], op=ALU.mult), reads=[tgu2, tgu], writes=[tge])
                    if kv == "k":
                        S.op("pe", lambda e: e.matmul(psX[:, 0:256], lhsT=w2["k"][0][:], rhs=ge[:], start=True, stop=True), reads=[w2["k"][1], tge], writes=[tpsX])
                        S.op("act", lambda e: e.copy(out=kcT[:], in_=psX[:, 0:256]), reads=[tpsX], writes=[tkcT])
                    else:
                        S.op("dve", lambda e: e.memset(vca[:], 1.0), writes=[tvca])
                        for kt in range(2):
                            S.op("pe", lambda e, kt=kt: e.matmul(psX[:, kt * 64:(kt + 1) * 64], lhsT=ge[:, kt * 128:(kt + 1) * 128], rhs=w2["v"][0][:], start=True, stop=True),
                                 reads=[w2["v"][1], tge], writes=[tpsX])
                        S.op("act", lambda e: e.copy(out=vca[:, :, 0:64], in_=psX[:, 0:128].rearrange("p (a b) -> p a b", b=64)), reads=[tpsX], writes=[tvca])
                pipe = []

                def submit(score_args, pv_fn, post_fn=None, extra=None, cr=(0, 512)):
                    ei = score_tile(*score_args, extra=extra, cr=cr)
                    if pipe:
                        ppv, ppost = pipe.pop(0)
                        ppv()
                        if ppost is not None:
                            ppost()
                    pipe.append((lambda ei=ei: pv_fn(ei), post_fn))

                def drain():
                    while pipe:
                        ppv, ppost = pipe.pop(0)
                        ppv()
                        if ppost is not None:
                            ppost()

                accs = [(psOS, tpsOS), (psOW, tpsOW)]

                def phase_A(qb):
                    qs = slice(qb * 512, (qb + 1) * 512)
                    oa, toa = oacc[qb % 2], toacc[qb % 2]
                    for h in range(4):
                        hp = (h % 2) * 64
                        q_ap = qT[h // 2][hp:hp + 64, qs]
                        kts = [0] if qb < 4 else [0, 1]
                        if h % 2 == 0:
                            pa, tpa, pb, tpb = psCA, tpsCA, psCB, tpsCB
                        else:
                            pa, tpa, pb, tpb = psOS, tpsOS, psOW, tpsOW

                        def pv(ei, kt, pa=pa, tpa=tpa, pb=pb, tpb=tpb, kts=kts):
                            for tc in range(4):
                                S.op("pe", lambda e: e.matmul(pa[:, tc * 65:(tc + 1) * 65], lhsT=Eb[ei][:, tc * 128:(tc + 1) * 128], rhs=vca[:, kt, 0:65], start=(kt == 0 and tc == 0), stop=(kt == kts[-1]), skip_group_check=True),
                                     reads=[tE[ei], tvca], writes=[tpa])
                            for tc in range(4):
                                S.op("pe", lambda e: e.matmul(pb[:, tc * 64:(tc + 1) * 64], lhsT=Eb[ei][:, tc * 128:(tc + 1) * 128], rhs=aggb[:, kt, :], start=(kt == 0 and tc == 0), stop=(kt == kts[-1]), skip_group_check=True),
                                     reads=[tE[ei], tagg], writes=[tpb])

                        def post(h=h, pa=pa, tpa=tpa, pb=pb, tpb=tpb):
                            cav = pa[:, 0:260].rearrange("p (a b) -> p a b", b=65)
                            S.op("dve", lambda e: e.tensor_scalar(out=rc4[:], in0=cav[:, :, 64], scalar1=1e-30, scalar2=None, op0=ALU.add), reads=[tpa], writes=[trc4])
                            S.op("dve", lambda e: e.reciprocal(out=rc4[:], in_=rc4[:]), reads=[trc4], writes=[trc4])
                            S.op("dve", lambda e: e.tensor_tensor(out=cf4[:], in0=rc4[:], in1=gt[:, qb * 4:(qb + 1) * 4, 0 * 8 + g * 4 + h], op=ALU.mult), reads=[trc4, tgt], writes=[tcf4])
                            S.op("dve", lambda e: e.tensor_tensor(out=oa[:, :, h, :], in0=cav[:, :, 0:64], in1=cf4[:].unsqueeze(2).to_broadcast([128, 4, 64]), op=ALU.mult),
                                 reads=[tpa, tcf4], writes=[toa])
                            cbv = pb[:, 0:256].rearrange("p (a b) -> p a b", b=64)
                            if h == 0:
                                S.op("dve", lambda e: e.tensor_tensor(out=imp[:], in0=cbv, in1=rc4[:].unsqueeze(2).to_broadcast([128, 4, 64]), op=ALU.mult), reads=[tpb, trc4], writes=[timp])
                            else:
                                S.op("dve", lambda e: e.tensor_tensor(out=tmp4[:], in0=cbv, in1=rc4[:].unsqueeze(2).to_broadcast([128, 4, 64]), op=ALU.mult), reads=[tpb, trc4], writes=[ttmp4])
                                S.op("dve", lambda e: e.tensor_tensor(out=imp[:], in0=imp[:], in1=tmp4[:], op=ALU.add), reads=[timp, ttmp4], writes=[timp])

                        for kt in kts:
                            off = min(512 * qb - 2048 * kt, 2560)
                            submit((kcT[hp:hp + 64, kt * 128:(kt + 1) * 128], [tkcT], q_ap, [tq[h // 2]], tabC[:, h, off:off + 512], [ttC]),
                                   lambda ei, kt=kt, pv=pv: pv(ei, kt), post if kt == kts[-1] else None)

                def phase_B1(qb):
                    for tc in range(4):
                        T4 = qb * 4 + tc
                        S.op("dve", lambda e: e.tensor_tensor(out=sc[:], in0=imp[:, tc, :], in1=selb[:, T4, :], op=ALU.add), reads=[timp, tselb], writes=[tsc])
                        S.op("dve", lambda e: e.max(out=m8[:], in_=sc[:]), reads=[tsc], writes=[tm8])
                        S.op("dve", lambda e: e.match_replace(out=sc2[:], in_to_replace=m8[:], in_values=sc[:], imm_value=-2e9), reads=[tsc, tm8], writes=[tsc2])
                        S.op("dve", lambda e: e.max(out=m8[:], in_=sc2[:]), reads=[tsc2], writes=[tm8])
                        S.op("dve", lambda e: e.tensor_scalar(out=nsel[:, tc, :], in0=sc[:], scalar1=m8[:, 7:8], scalar2=NEGM, op0=ALU.is_lt, op1=ALU.mult), reads=[tsc, tm8], writes=[tnsel])

                def phase_B2(qb):
                    for tc in range(4):
                        S.op("pe", lambda e: e.transpose(out=psT[0:64, tc * 128:(tc + 1) * 128], in_=nsel[:, tc, :], identity=idf[:]), reads=[tnsel, tidf], writes=[tpsT])
                    S.op("act", lambda e: e.copy(out=nselT[:], in_=psT[0:64, :]), reads=[tpsT], writes=[tnselT])

                def combine_post(qb, h, pso, tpso, br):
                    oa, toa = oacc[qb % 2], toacc[qb % 2]

                    def post():
                        ov = pso[:, 0:260].rearrange("p (a b) -> p a b", b=65)
                        S.op("dve", lambda e: e.reciprocal(out=rc4[:], in_=ov[:, :, 64]), reads=[tpso], writes=[trc4])
                        S.op("dve", lambda e: e.tensor_tensor(out=cf4[:], in0=rc4[:], in1=gt[:, qb * 4:(qb + 1) * 4, br * 8 + g * 4 + h], op=ALU.mult), reads=[trc4, tgt], writes=[tcf4])
                        S.op("dve", lambda e: e.tensor_tensor(out=tmp4[:], in0=ov[:, :, 0:64], in1=cf4[:].unsqueeze(2).to_broadcast([128, 4, 64]), op=ALU.mult), reads=[tpso, tcf4], writes=[ttmp4])
                        S.op("pool", lambda e: e.tensor_tensor(out=oa[:, :, h, :], in0=oa[:, :, h, :], in1=tmp4[:], op=ALU.add), reads=[toa, ttmp4], writes=[toa])
                    return post

                def phase_W(qb):
                    qs = slice(qb * 512, (qb + 1) * 512)
                    for h in range(4):
                        hp = (h % 2) * 64
                        q_ap = qT[h // 2][hp:hp + 64, qs]
                        pso, tpso = accs[h % 2]
                        kts = list(range(max(0, 4 * qb - 4), 4 * qb + 4))

                        def pv(ei, kt, pso=pso, tpso=tpso, kts=kts):
                            for tc in range(4):
                                T4 = 4 * qb + tc
                                lo = max(0, T4 - 4)
                                if kt > T4 or kt < lo:
                                    continue
                                S.op("pe", lambda e: e.matmul(pso[:, tc * 65:(tc + 1) * 65], lhsT=Eb[ei][:, tc * 128:(tc + 1) * 128], rhs=vw[:, kt, 0:65], start=(kt == kts[0] and tc == 0), stop=(kt == T4), skip_group_check=True),
                                     reads=[tE[ei], tvw], writes=[tpso])

                        for kt in kts:
                            off = 512 * qb - 128 * kt + 384
                            submit((kwT[hp:hp + 64, kt * 128:(kt + 1) * 128], [tkw], q_ap, [tq[h // 2]], tabW[:, h, off:off + 512], [ttW]),
                                   lambda ei, kt=kt, pv=pv: pv(ei, kt), combine_post(qb, h, pso, tpso, 2) if kt == kts[-1] else None,
                                   cr=(max(0, kt - 4 * qb) * 128, (min(3, kt + 4 - 4 * qb) + 1) * 128))

                def phase_S(qb):
                    qs = slice(qb * 512, (qb + 1) * 512)
                    for h in range(4):
                        hp = (h % 2) * 64
                        q_ap = qT[h // 2][hp:hp + 64, qs]
                        pso, tpso = accs[h % 2]
                        kts = list(range(4 * qb + 4))

                        def pv(ei, kt, pso=pso, tpso=tpso):
                            for tc in range(4):
                                T4 = 4 * qb + tc
                                if kt > T4:
                                    continue
                                S.op("pe", lambda e: e.matmul(pso[:, tc * 65:(tc + 1) * 65], lhsT=Eb[ei][:, tc * 128:(tc + 1) * 128], rhs=vs[:, kt, 0:65], start=(kt == 0 and tc == 0), stop=(kt == T4), skip_group_check=True),
                                     reads=[tE[ei], tvs], writes=[tpso])

                        for kt in kts:
                            off = min(512 * qb - 128 * kt, 256) + 384
                            submit((ksT[hp:hp + 64, kt * 128:(kt + 1) * 128], [tks], q_ap, [tq[h // 2]], tabS[:, h, off:off + 512], [ttS]),
                                   lambda ei, kt=kt, pv=pv: pv(ei, kt), combine_post(qb, h, pso, tpso, 1) if kt == kts[-1] else None,
                                   extra=(esel[:, kt, :], nselT[:], [tesel, tnselT]), cr=(max(0, kt - 4 * qb) * 128, 512))

                def phase_F(qb):
                    oa, toa = oacc[qb % 2], toacc[qb % 2]
                    for c2 in range(2):
                        for tc in range(4):
                            S.op("pe", lambda e: e.transpose(out=psT[:, tc * 128:(tc + 1) * 128], in_=oa[:, tc, 2 * c2:2 * c2 + 2, :].rearrange("p a b -> p (a b)"), identity=idf[:]),
                                 reads=[toa, tidf], writes=[tpsT])
                        S.op("act", lambda e: e.copy(out=ostg[:, c2, :], in_=psT[:]), reads=[tpsT], writes=[tostg])
                    S.dma("sp", k.catT[4 + 2 * g:6 + 2 * g, :, s * SEQ + qb * 512:s * SEQ + (qb + 1) * 512].rearrange("c p t -> p c t"), ostg[:], reads=[tostg])

                for qb in range(8):
                    phase_A(qb)
                    drain()
                    if qb > 0:
                        phase_F(qb - 1)
                    phase_B1(qb)
                    phase_W(qb)
                    drain()
                    phase_B2(qb)
                    phase_S(qb)
                    drain()
                phase_F(7)
        S.barrier()


def layer_norm_rows(k, y, ty, dst, tdst, gam, tgam, bet, tbet, st, tst, mv, tmv):
    S = k.S
    for q in range(2):
        S.op("dve", lambda e, q=q: e.bn_stats(out=st[:, q, :], in_=y[:, q * 512:(q + 1) * 512]), reads=[ty], writes=[tst])
    S.op("dve", lambda e: e.bn_aggr(out=mv[:, 0:2], in_=st[:].rearrange("p a b -> p (a b)")), reads=[tst], writes=[tmv])
    S.op("dve", lambda e: e.tensor_scalar(out=mv[:, 2:3], in0=mv[:, 1:2], scalar1=1e-5, scalar2=None, op0=ALU.add), reads=[tmv], writes=[tmv])
    S.op("act", lambda e: e.activation(out=mv[:, 2:3], in_=mv[:, 2:3], func=AF.Sqrt), reads=[tmv], writes=[tmv])
    S.op("dve", lambda e: e.reciprocal(out=mv[:, 2:3], in_=mv[:, 2:3]), reads=[tmv], writes=[tmv])
    S.op("dve", lambda e: e.tensor_scalar(out=y, in0=y, scalar1=mv[:, 0:1], scalar2=mv[:, 2:3], op0=ALU.subtract, op1=ALU.mult), reads=[ty, tmv], writes=[ty])
    S.op("pool", lambda e: e.tensor_tensor(out=y, in0=y, in1=gam, op=ALU.mult), reads=[ty, tgam], writes=[ty])
    S.op("pool", lambda e: e.tensor_tensor(out=dst, in0=y, in1=bet, op=ALU.add), reads=[ty, tbet], writes=[tdst])


def load_bcast(k, es, name, src_row):
    nc, S = k.nc, k.S
    t = _sb(es, nc, name, [128, D], F32)
    tt = T(name)
    S.dma("sp", t[:], src_row.rearrange("(o d) -> o d", o=1).to_broadcast([128, D]), writes=[tt])
    return t, tt


def stage_out(k, l, xin):
    nc, S = k.nc, k.S
    NT = k.NT
    with ExitStack() as es:
        wsb = _sb(es, nc, "o_w", [128, 8, D], BF16)
        tw = [T("w%d" % i) for i in range(8)]
        for kc in range(8):
            S.dma("pool", wsb[:, kc, :], k.w["w_out"][l, kc * 128:(kc + 1) * 128, :], writes=[tw[kc]])
        gam, tgam = load_bcast(k, es, "o_gam", k.w["ln1_g"][l])
        bet, tbet = load_bcast(k, es, "o_bet", k.w["ln1_b"][l])
        cT = [_sb(es, nc, "o_cT%d" % i, [128, 8, 512], BF16) for i in range(2)]; tcT = [T("cT%d" % i) for i in range(2)]
        xt = [_sb(es, nc, "o_xt%d" % i, [128, 4, D], F32) for i in range(2)]; txt = [T("xt%d" % i) for i in range(2)]
        y = [_sb(es, nc, "o_y%d" % i, [128, D], F32) for i in range(2)]; ty = [T("y%d" % i) for i in range(2)]
        yo = [_sb(es, nc, "o_yo%d" % i, [128, 4, D], F32) for i in range(2)]; tyo = [T("yo%d" % i) for i in range(2)]
        st = _sb(es, nc, "o_st", [128, 2, 6], F32); tst = T("st")
        mv = _sb(es, nc, "o_mv", [128, 4], F32); tmv = T("mv")
        ps = [_ps(es, nc, "o_ps%d" % i, [128, 512], F32) for i in range(4)]; tps = [T("ps%d" % i) for i in range(4)]
        nblk = NT // 512

        def load(b):
            bi = b % 2
            S.dma("sp", cT[bi][:], k.catT[:, :, b * 512:(b + 1) * 512].rearrange("c p t -> p c t"), writes=[tcT[bi]])
            S.dma("sp", xt[bi][:], xin[b * 512:(b + 1) * 512, :].rearrange("(j p) d -> p j d", p=128), writes=[txt[bi]])

        load(0)
        pc = 0
        for b in range(nblk):
            bi = b % 2
            if b + 1 < nblk:
                load(b + 1)
            for j in range(4):
                yi = j % 2
                for half in range(2):
                    pi = pc % 4
                    pc += 1
                    for kc in range(8):
                        S.op("pe", lambda e, j=j, kc=kc, pi=pi, half=half: e.matmul(ps[pi][:], lhsT=cT[bi][:, kc, j * 128:(j + 1) * 128], rhs=wsb[:, kc, half * 512:(half + 1) * 512], start=(kc == 0), stop=(kc == 7)),
                             reads=[tcT[bi], tw[kc]], writes=[tps[pi]])
                    S.op("dve", lambda e, j=j, pi=pi, half=half, yi=yi: e.scalar_tensor_tensor(out=y[yi][:, half * 512:(half + 1) * 512], in0=xt[bi][:, j, half * 512:(half + 1) * 512], scalar=ALPHA, in1=ps[pi][:], op0=ALU.mult, op1=ALU.add),
                         reads=[txt[bi], tps[pi]], writes=[ty[yi]])
                layer_norm_rows(k, y[yi][:], ty[yi], yo[bi][:, j, :], tyo[bi], gam[:], tgam, bet[:], tbet, st, tst, mv, tmv)
            S.dma("sp", k.x1[b * 512:(b + 1) * 512, :].rearrange("(j p) d -> p j d", p=128), yo[bi][:], reads=[tyo[bi]])
        S.barrier()


def stage_ffn(k, l, xout):
    nc, S = k.nc, k.S
    NT = k.NT
    NB = 256
    nj = NB // 128
    NF = D_FF // 128
    with ExitStack() as es:
        cb = load_consts_bf(k, es, ["ident"])
        ident, tid = cb["ident"]
        wg = _sb(es, nc, "f_wg", [128, 8, D_FF], BF16); twg = [T("wg%d" % i) for i in range(8)]
        wu = _sb(es, nc, "f_wu", [128, 8, D_FF], BF16); twu = [T("wu%d" % i) for i in range(8)]
        wd = _sb(es, nc, "f_wd", [128, NF, D], BF16); twd = [T("wd%d" % i) for i in range(NF)]
        for kc in range(8):
            S.dma("pool", wg[:, kc, :], k.w["w_ffn_gate"][l, kc * 128:(kc + 1) * 128, :], writes=[twg[kc]])
            S.dma("pool", wu[:, kc, :], k.w["w_ffn_up"][l, kc * 128:(kc + 1) * 128, :], writes=[twu[kc]])
        for f in range(NF):
            S.dma("pool", wd[:, f, :], k.w["w_ffn_down"][l, f * 128:(f + 1) * 128, :], writes=[twd[f]])
        gam, tgam = load_bcast(k, es, "f_gam", k.w["ln2_g"][l])
        bet, tbet = load_bcast(k, es, "f_bet", k.w["ln2_b"][l])
        xt = [_sb(es, nc, "f_xt%d" % i, [128, nj, D], F32) for i in range(2)]; txt = [T("xt%d" % i) for i in range(2)]
        xb = _sb(es, nc, "f_xb", [128, nj, D], BF16); txb = T("xb")
        xT = _sb(es, nc, "f_xT", [128, 8, NB], BF16); txT = [T("xT%d" % i) for i in range(8)]
        hT = _sb(es, nc, "f_hT", [128, NF, NB], BF16); thT = [T("hT%d" % i) for i in range(NF)]
        sg = [_sb(es, nc, "f_sg%d" % i, [128, NB], F32) for i in range(2)]; tsg = [T("sg%d" % i) for i in range(2)]
        y = [_sb(es, nc, "f_y%d" % i, [128, D], F32) for i in range(2)]; ty = [T("y%d" % i) for i in range(2)]
        yo = [_sb(es, nc, "f_yo%d" % i, [128, nj, D], F32) for i in range(2)]; tyo = [T("yo%d" % i) for i in range(2)]
        st = _sb(es, nc, "f_st", [128, 2, 6], F32); tst = T("st")
        mv = _sb(es, nc, "f_mv", [128, 4], F32); tmv = T("mv")
        psT = [_ps(es, nc, "f_psT%d" % i, [128, 1024], BF16) for i in range(2)]; tpsT = [T("psT%d" % i) for i in range(2)]
        psg = [_ps(es, nc, "f_psg%d" % i, [128, 512], F32) for i in range(2)]; tpsg = [T("psg%d" % i) for i in range(2)]
        psu = [_ps(es, nc, "f_psu%d" % i, [128, 512], F32) for i in range(2)]; tpsu = [T("psu%d" % i) for i in range(2)]
        psd = [_ps(es, nc, "f_psd%d" % i, [128, 512], F32) for i in range(2)]; tpsd = [T("psd%d" % i) for i in range(2)]
        nblk = NT // NB
        ctr = [0]

        def load(b):
            S.dma("sp", xt[b % 2][:], k.x1[b * NB:(b + 1) * NB, :].rearrange("(j p) d -> p j d", p=128), writes=[txt[b % 2]])

        load(0)
        pc = 0
        for b in range(nblk):
            bi = b % 2
            if b + 1 < nblk:
                load(b + 1)
            S.op("pool", lambda e: e.tensor_copy(out=xb[:], in_=xt[bi][:]), reads=[txt[bi]], writes=[txb])
            transpose_block(k, xb, txb, xT, txT, ident[:], tid, psT, tpsT, NB, ctr)
            for f in range(NF):
                pi = f % 2
                for kc in range(8):
                    S.op("pe", lambda e, f=f, kc=kc, pi=pi: e.matmul(psg[pi][:, 0:NB], lhsT=wg[:, kc, f * 128:(f + 1) * 128], rhs=xT[:, kc, :], start=(kc == 0), stop=(kc == 7)),
                         reads=[twg[kc], txT[kc]], writes=[tpsg[pi]])
                for kc in range(8):
                    S.op("pe", lambda e, f=f, kc=kc, pi=pi: e.matmul(psu[pi][:, 0:NB], lhsT=wu[:, kc, f * 128:(f + 1) * 128], rhs=xT[:, kc, :], start=(kc == 0), stop=(kc == 7)),
                         reads=[twu[kc], txT[kc]], writes=[tpsu[pi]])
                S.op("act", lambda e, pi=pi: e.activation(out=sg[pi][:], in_=psg[pi][:, 0:NB], func=AF.Silu), reads=[tpsg[pi]], writes=[tsg[pi]])
                S.op("dve", lambda e, f=f, pi=pi: e.tensor_tensor(out=hT[:, f, :], in0=sg[pi][:], in1=psu[pi][:, 0:NB], op=ALU.mult), reads=[tsg[pi], tpsu[pi]], writes=[thT[f]])
            for j in range(nj):
                yi = j % 2
                for half in range(2):
                    pi = pc % 2
                    pc += 1
                    for f in range(NF):
                        S.op("pe", lambda e, j=j, f=f, pi=pi, half=half: e.matmul(psd[pi][:], lhsT=hT[:, f, j * 128:(j + 1) * 128], rhs=wd[:, f, half * 512:(half + 1) * 512], start=(f == 0), stop=(f == NF - 1)),
                             reads=[thT[f], twd[f]], writes=[tpsd[pi]])
                    S.op("dve", lambda e, j=j, pi=pi, half=half, yi=yi: e.scalar_tensor_tensor(out=y[yi][:, half * 512:(half + 1) * 512], in0=xt[bi][:, j, half * 512:(half + 1) * 512], scalar=ALPHA, in1=psd[pi][:], op0=ALU.mult, op1=ALU.add),
                         reads=[txt[bi], tpsd[pi]], writes=[ty[yi]])
                layer_norm_rows(k, y[yi][:], ty[yi], yo[bi][:, j, :], tyo[bi], gam[:], tgam, bet[:], tbet, st, tst, mv, tmv)
            S.dma("sp", xout[b * NB:(b + 1) * NB, :].rearrange("(j p) d -> p j d", p=128), yo[bi][:], reads=[tyo[bi]])
        S.barrier()


def kernel(**inputs):
    x = np.ascontiguousarray(np.asarray(inputs["x"], dtype=np.float32))
    B = x.shape[0]
    nseq = B // N_CORES
    consts = _consts(np.asarray(inputs["rel_bias"], dtype=np.float32))
    kk = build(nseq=nseq, depth=DEPTH)
    base = {n: np.ascontiguousarray(np.asarray(inputs[n], dtype=np.float32)) for n in kk.wnames}
    for n, v in consts.items():
        base["c_" + n] = np.ascontiguousarray(v.astype(np.float32))
    in_maps = []
    for c in range(N_CORES):
        m = dict(base)
        m["x"] = x[c * nseq:(c + 1) * nseq].reshape(nseq * SEQ, D)
        in_maps.append(m)
    res = run_bass_kernel_spmd(kk.nc, in_maps, core_ids=list(range(N_CORES)))
    outs = [np.asarray(r["out"]).reshape(nseq, SEQ, D) for r in res.results]
    return np.concatenate(outs, axis=0).astype(np.float32)
```

```python
import math
from contextlib import ExitStack
import numpy as np
import concourse.bass as bass
import concourse.mybir as mybir
from concourse.bass_utils import run_bass_kernel_spmd

F32 = mybir.dt.float32
BF16 = mybir.dt.bfloat16
AF = mybir.ActivationFunctionType
ALU = mybir.AluOpType

D = 1024
SEQ = 4096
DEPTH = 4
D_IN = 3352
D_FF = 2816
ALPHA = (2.0 * DEPTH) ** 0.25
NEGM = -30000.0
N_CORES = 8


class T:
    __slots__ = ("name", "w", "r")

    def __init__(self, name):
        self.name = name
        self.w = None
        self.r = []


class Sched:
    ENG = ("pe", "act", "dve", "pool", "sp")

    def __init__(self, nc, ndma=(14, 4, 14)):
        self.nc = nc
        self.e = {"pe": nc.tensor, "act": nc.scalar, "dve": nc.vector, "pool": nc.gpsimd, "sp": nc.sync}
        self.sems = {}
        self.cnt = {}
        for k in self.ENG:
            self.sems[k] = nc.alloc_semaphore("s_" + k)
            self.cnt[k] = 0
        self.dq = {}
        for q, n in zip(("sp", "act", "pool"), ndma):
            lst = []
            for i in range(n):
                key = "d_%s_%d" % (q, i)
                self.sems[key] = nc.alloc_semaphore(key)
                self.cnt[key] = 0
                lst.append(key)
            self.dq[q] = [lst, 0]
        self.seen = {k: {} for k in self.ENG}
        self.ninst = 0
        self.nwait = 0

    def _wait(self, eng, ev):
        if ev is None:
            return
        key, val = ev
        if key == eng and eng == "pe":
            return
        if self.seen[eng].get(key, 0) >= val:
            return
        self.e[eng].wait_ge(self.sems[key], val)
        self.seen[eng][key] = val
        self.nwait += 1

    def _deps(self, eng, reads, writes):
        for t in reads:
            self._wait(eng, t.w)
        for t in writes:
            self._wait(eng, t.w)
            for ev in t.r:
                self._wait(eng, ev)

    def _commit(self, ev, reads, writes):
        for t in reads:
            t.r.append(ev)
            if len(t.r) > 16:
                best = {}
                for k, v in t.r:
                    if best.get(k, 0) < v:
                        best[k] = v
                t.r = list(best.items())
        for t in writes:
            t.w = ev
            t.r = []

    def op(self, eng, fn, reads=(), writes=()):
        self._deps(eng, reads, writes)
        ins = fn(self.e[eng])
        self.cnt[eng] += 1
        ins.then_inc(self.sems[eng], 1)
        self._commit((eng, self.cnt[eng]), reads, writes)
        self.ninst += 1
        return ins

    def dma(self, q, out, in_, reads=(), writes=(), **kw):
        lst, idx = self.dq[q]
        key = lst[idx % len(lst)]
        self.dq[q][1] = idx + 1
        if self.cnt[key] > 0:
            self._wait(q, (key, self.cnt[key]))
        self._deps(q, reads, writes)
        ins = self.e[q].dma_start(out=out, in_=in_, **kw)
        self.cnt[key] += 16
        ins.then_inc(self.sems[key], 16)
        self._commit((key, self.cnt[key]), reads, writes)
        self.ninst += 1
        return ins

    def barrier(self, engines=None):
        for eng in (engines or self.ENG):
            for key, c in self.cnt.items():
                if c > 0:
                    self._wait(eng, (key, c))


def _t5_bucket(n):
    n = np.maximum(n, 0)
    nf = np.maximum(n, 1).astype(np.float32)
    large = 16 + (np.log(nf / np.float32(16.0)) / np.float32(math.log(8.0)) * np.float32(16.0)).astype(np.int32)
    large = np.minimum(large, 31)
    return np.where(n < 16, n, large)


WS, WW, WC = 1152, 1408, 3072


def _consts(rel_bias):
    c = {}
    c["ident"] = np.eye(128, dtype=np.float32)
    s = np.arange(64)[:, None]
    t = np.arange(512)[None, :] % 64
    c["caus"] = (s <= t).astype(np.float32)
    seg = np.ones((128, SEQ), np.float32)
    seg[:, ::64] = 0.0
    c["seg"] = seg
    c["ones"] = np.ones((128, 128), np.float32)
    es = np.zeros((64, 32, 128), np.float32)
    for kt in range(32):
        es[2 * kt, kt, :64] = 1.0
        es[2 * kt + 1, kt, 64:] = 1.0
    c["esel"] = es
    tpos = np.arange(SEQ)
    cur = tpos // 64
    j = np.arange(64)[None, :]
    diff = cur[:, None] - j
    forced = (j == 0) | ((diff >= 0) & (diff < 2))
    sb = np.where(forced, 1e9, np.where(j <= cur[:, None], 0.0, -1e9)).astype(np.float32)
    c["selb"] = sb
    cs = np.arange(256) * 16
    ss = np.arange(64) * 64
    ov = np.clip(np.minimum(cs[:, None] + 32, ss[None, :] + 64) - np.maximum(cs[:, None], ss[None, :]), 0, None)
    agg = (ov / 16.0).astype(np.float32)
    agg[255] = 0.0
    c["agg"] = agg.reshape(2, 128, 64).transpose(1, 0, 2).copy()
    kl = np.arange(128)[:, None]
    col = np.arange(WW)[None, :]
    dist = col - 384 - kl
    bk = _t5_bucket(dist)
    rb = np.asarray(rel_bias, np.float32)
    c["tb"] = np.ascontiguousarray(rb[bk].transpose(2, 0, 1))
    c["mS"] = np.where(dist[:, :WS] < 0, NEGM, 0.0).astype(np.float32)
    c["mW"] = np.where((dist < 0) | (dist >= 512), NEGM, 0.0).astype(np.float32)
    colc = np.arange(WC)[None, :]
    distc = colc - 16 * kl - 31
    bkc = _t5_bucket(distc)
    c["tbc"] = np.ascontiguousarray(rb[bkc].transpose(2, 0, 1))
    c["mC"] = np.where(distc < 0, NEGM, 0.0).astype(np.float32)
    return c


CONST_SHAPES = {
    "ident": [128, 128], "caus": [64, 512], "seg": [128, SEQ], "ones": [128, 128], "esel": [64, 32, 128],
    "selb": [SEQ, 64], "agg": [128, 2, 64], "tb": [8, 128, WW], "mS": [128, WS], "mW": [128, WW],
    "tbc": [8, 128, WC], "mC": [128, WC],
}

WEIGHT_SHAPES = {
    "w_in": [DEPTH, D, D_IN], "hg_lb_param": [DEPTH, 512], "hg_norm_w": [DEPTH, 128],
    "cmp_pos_k": [DEPTH, 32, 64], "cmp_w1_k": [DEPTH, 2048, 128], "cmp_w2_k": [DEPTH, 128, 64],
    "cmp_pos_v": [DEPTH, 32, 64], "cmp_w1_v": [DEPTH, 2048, 128], "cmp_w2_v": [DEPTH, 128, 64],
    "w_out": [DEPTH, D, D], "ln1_g": [DEPTH, D], "ln1_b": [DEPTH, D],
    "w_ffn_gate": [DEPTH, D, D_FF], "w_ffn_up": [DEPTH, D, D_FF], "w_ffn_down": [DEPTH, D_FF, D],
    "ln2_g": [DEPTH, D], "ln2_b": [DEPTH, D],
}


class K:
    pass


_UID = [0]


def _sb(es, nc, name, shape, dt):
    _UID[0] += 1
    return es.enter_context(nc.sbuf_tensor("%s_u%d" % (name, _UID[0]), shape, dt))


def _ps(es, nc, name, shape, dt):
    _UID[0] += 1
    return es.enter_context(nc.psum_tensor("%s_u%d" % (name, _UID[0]), shape, dt))


def build(nseq=2, depth=DEPTH, dbg=False, stages="PHNOF", ntok=None):
    nc = bass.Bass("TRN2", target_bir_lowering=False)
    k = K()
    k.nc = nc
    k.nseq = nseq
    NT = ntok or nseq * SEQ
    k.NT = NT
    k.S = Sched(nc)
    k.x = nc.dram_tensor("x", [NT, D], F32, kind="ExternalInput").ap()
    k.out = nc.dram_tensor("out", [NT, D], F32, kind="ExternalOutput").ap()
    need = {"P": ["w_in"], "H": ["hg_norm_w"], "N": ["cmp_pos_k", "cmp_w1_k", "cmp_w2_k", "cmp_pos_v", "cmp_w1_v", "cmp_w2_v"],
            "O": ["w_out", "ln1_g", "ln1_b"], "F": ["w_ffn_gate", "w_ffn_up", "w_ffn_down", "ln2_g", "ln2_b"]}
    wn = ["hg_lb_param"] + [n for st in stages if st in need for n in need[st]]
    k.wnames = wn
    k.w = {n: nc.dram_tensor(n, [DEPTH if n == "hg_lb_param" else depth] + list(WEIGHT_SHAPES[n][1:]), F32, kind="ExternalInput").ap() for n in wn}
    k.c = {n: nc.dram_tensor("c_" + n, s, F32, kind="ExternalInput").ap() for n, s in CONST_SHAPES.items()}
    ikind = "ExternalOutput" if dbg else "Internal"
    k.fm = nc.dram_tensor("fm", [27, 128, NT], BF16, kind=ikind).ap()
    k.fmf = nc.dram_tensor("fmf", [4, 128, NT], F32, kind=ikind).ap()
    k.tmi = nc.dram_tensor("tmi", [NT, 512], BF16, kind=ikind).ap()
    k.tmv = nc.dram_tensor("tmv", [NT, 256], BF16, kind=ikind).ap()
    k.tmg = nc.dram_tensor("tmg", [NT, 24], F32, kind=ikind).ap()
    k.catT = nc.dram_tensor("catT", [8, 128, NT], BF16, kind=ikind).ap()
    k.x1 = nc.dram_tensor("x1", [NT, D], F32, kind=ikind).ap()
    k.xa = nc.dram_tensor("xa", [NT, D], F32, kind="Internal").ap()
    k.xb = nc.dram_tensor("xb", [NT, D], F32, kind="Internal").ap()
    k.lbs = nc.dram_tensor("lbs", [DEPTH, 512], F32, kind="Internal").ap()
    k.tS = nc.dram_tensor("tS", [8, 128, WS], BF16, kind="Internal").ap()
    k.tW = nc.dram_tensor("tW", [8, 128, WW], BF16, kind="Internal").ap()
    k.tC = nc.dram_tensor("tC", [8, 128, WC], BF16, kind="Internal").ap()

    if "X" not in stages:
        stage_setup(k)
        k.S.barrier()
    xin = k.x
    for l in range(depth):
        last = l == depth - 1
        xout = k.out if last else (k.xa if l % 2 == 0 else k.xb)
        if "P" in stages:
            stage_proj(k, l, xin)
            k.S.barrier()
        if "H" in stages:
            stage_hgrn(k, l)
            k.S.barrier()
        if "N" in stages:
            stage_nsa(k, l)
            k.S.barrier()
        if "O" in stages:
            stage_out(k, l, xin)
            k.S.barrier()
        if "F" in stages:
            stage_ffn(k, l, xout)
            k.S.barrier()
        xin = xout
    k.S.barrier(["sp"])
    return k


def stage_setup(k):
    nc, S = k.nc, k.S
    with ExitStack() as es:
        lp = _sb(es, nc, "su_lp", [128, DEPTH, 4], F32)
        tl = T("lp")
        S.dma("sp", lp[:], k.w["hg_lb_param"].rearrange("l (q p) -> p l q", p=128), writes=[tl],
              allow_slow_non_contiguous=True)
        ex = _sb(es, nc, "su_ex", [128, DEPTH, 4], F32)
        te = T("ex")
        S.op("act", lambda e: e.activation(out=ex[:], in_=lp[:], func=AF.Exp), reads=[tl], writes=[te])
        sm = _sb(es, nc, "su_sm", [128, 4], F32)
        ts = T("sm")
        S.op("dve", lambda e: e.tensor_tensor(out=sm[:], in0=ex[:, 0, :], in1=ex[:, 1, :], op=ALU.add), reads=[te], writes=[ts])
        for l in range(2, DEPTH):
            S.op("dve", lambda e, l=l: e.tensor_tensor(out=sm[:], in0=sm[:], in1=ex[:, l, :], op=ALU.add), reads=[te, ts], writes=[ts])
        S.op("dve", lambda e: e.reciprocal(out=sm[:], in_=sm[:]), reads=[ts], writes=[ts])
        lb = _sb(es, nc, "su_lb", [128, DEPTH, 4], F32)
        tb_ = T("lb")
        S.op("dve", lambda e: e.memset(lb[:], 0.0), writes=[tb_])
        for l in range(1, DEPTH):
            S.op("dve", lambda e, l=l: e.tensor_tensor(out=ex[:, l, :], in0=ex[:, l, :], in1=sm[:], op=ALU.mult), reads=[te, ts], writes=[te])
            S.op("dve", lambda e, l=l: e.tensor_tensor(out=lb[:, l, :], in0=lb[:, l - 1, :], in1=ex[:, l, :], op=ALU.add), reads=[te, tb_], writes=[tb_])
        S.dma("sp", k.lbs.rearrange("l (q p) -> p l q", p=128), lb[:], reads=[tb_], allow_slow_non_contiguous=True)
        S.barrier()
    for (src, msk, dst, W) in (("tb", "mS", k.tS, WS), ("tb", "mW", k.tW, WW), ("tbc", "mC", k.tC, WC)):
        with ExitStack() as es:
            mt = _sb(es, nc, "su_m_" + msk, [128, W], F32)
            tm = T("m")
            S.dma("sp", mt[:], k.c[msk], writes=[tm])
            bt = [_sb(es, nc, "su_b_%s_%d" % (msk, i), [128, W], F32) for i in range(2)]
            ot = [_sb(es, nc, "su_o_%s_%d" % (msk, i), [128, W], BF16) for i in range(2)]
            tbt = [T("b0"), T("b1")]
            tot = [T("o0"), T("o1")]
            for h in range(8):
                i = h % 2
                S.dma("sp", bt[i][:], k.c[src][h, :, 0:W], writes=[tbt[i]])
                S.op("dve", lambda e: e.tensor_tensor(out=bt[i][:], in0=bt[i][:], in1=mt[:], op=ALU.add), reads=[tbt[i], tm], writes=[tbt[i]])
                S.op("act", lambda e: e.activation(out=ot[i][:], in_=bt[i][:], func=AF.Exp), reads=[tbt[i]], writes=[tot[i]])
                S.dma("sp", dst[h], ot[i][:], reads=[tot[i]])
            S.barrier()


PSKIP = ''
NSKIP = ''
FM_GROUPS = [[0, 1, 2, 3], [12, 13, 14, 15], [16, 17, 18, 19], [20, 21, 22], [24]]


def load_consts_bf(k, es, names):
    nc, S = k.nc, k.S
    out = {}
    for n in names:
        shp = CONST_SHAPES[n]
        t = _sb(es, nc, "cb_" + n, shp, BF16)
        tt = T("cb_" + n)
        S.dma("pool", t[:], k.c[n], writes=[tt])
        out[n] = (t, tt)
    return out


def transpose_block(k, xsrc, txsrc, xT, txT, ident, tid, psT, tpsT, ncols, ctr):
    S = k.S
    nj = ncols // 128
    for kc in range(8):
        pi = ctr[0] % len(psT)
        ctr[0] += 1
        for j in range(nj):
            S.op("pe", lambda e, j=j, kc=kc, pi=pi: e.transpose(out=psT[pi][:, j * 128:(j + 1) * 128], in_=xsrc[:, j, kc * 128:(kc + 1) * 128], identity=ident),
                 reads=[txsrc, tid], writes=[tpsT[pi]])
        eng = "act" if kc % 2 == 0 else "dve"
        if eng == "act":
            S.op("act", lambda e, kc=kc, pi=pi: e.copy(out=xT[:, kc, :], in_=psT[pi][:, 0:ncols]), reads=[tpsT[pi]], writes=[txT[kc]])
        else:
            S.op("dve", lambda e, kc=kc, pi=pi: e.tensor_copy(out=xT[:, kc, :], in_=psT[pi][:, 0:ncols]), reads=[tpsT[pi]], writes=[txT[kc]])


def stage_proj(k, l, xin):
    nc, S = k.nc, k.S
    NT = k.NT
    nblk = NT // 512
    with ExitStack() as es:
        cb = load_consts_bf(k, es, ["ident"])
        ident, tid = cb["ident"]
        wsb = _sb(es, nc, "p_w", [128, 8, D_IN], BF16)
        tw = [T("w%d" % i) for i in range(8)]
        for kc in range(8):
            S.dma("pool", wsb[:, kc, :], k.w["w_in"][l, kc * 128:(kc + 1) * 128, :], writes=[tw[kc]])
        xt = [_sb(es, nc, "p_xt%d" % i, [128, 4, D], F32) for i in range(2)]
        txt = [T("xt%d" % i) for i in range(2)]
        xb = _sb(es, nc, "p_xb", [128, 4, D], BF16)
        txb = T("xb")
        xT = [_sb(es, nc, "p_xT%d" % i, [128, 8, 512], BF16) for i in range(2)]
        txT = [[T("xT%d_%d" % (i, kc)) for kc in range(8)] for i in range(2)]
        stg = [_sb(es, nc, "p_stg%d" % i, [128, 27, 512], BF16) for i in range(2)]
        tstg = [[T("stg%d_%d" % (i, g)) for g in range(len(FM_GROUPS))] for i in range(2)]
        stf = [_sb(es, nc, "p_stf%d" % i, [128, 4, 512], F32) for i in range(2)]
        tstf = [T("stf%d" % i) for i in range(2)]
        sti = [_sb(es, nc, "p_sti%d" % i, [128, 4, 512], BF16) for i in range(2)]
        tsti = [T("sti%d" % i) for i in range(2)]
        stv = [_sb(es, nc, "p_stv%d" % i, [128, 4, 256], BF16) for i in range(2)]
        tstv = [T("stv%d" % i) for i in range(2)]
        stgt = [_sb(es, nc, "p_stgt%d" % i, [128, 4, 24], F32) for i in range(2)]
        tstgt = [T("stgt%d" % i) for i in range(2)]
        psT = [_ps(es, nc, "p_psT%d" % i, [128, 1024], BF16) for i in range(2)]
        tpsT = [T("psT%d" % i) for i in range(2)]
        psm = [_ps(es, nc, "p_psm%d" % i, [128, 512], F32) for i in range(5)]
        tpsm = [T("psm%d" % i) for i in range(5)]
        ctr = [0]
        pctr = [0]
        ectr = [0]

        def evac(dst, src, reads, writes, scale=None, eng=None):
            if eng is None:
                eng = "act" if ectr[0] % 2 == 0 else "dve"
                ectr[0] += 1
            if eng == "act":
                if scale is None:
                    S.op("act", lambda e: e.copy(out=dst, in_=src), reads=reads, writes=writes)
                else:
                    S.op("act", lambda e: e.mul(out=dst, in_=src, mul=scale), reads=reads, writes=writes)
            else:
                if scale is None:
                    S.op("dve", lambda e: e.tensor_copy(out=dst, in_=src), reads=reads, writes=writes)
                else:
                    S.op("dve", lambda e: e.tensor_scalar(out=dst, in0=src, scalar1=scale, scalar2=None, op0=ALU.mult), reads=reads, writes=writes)

        def load_x(b):
            S.dma("sp", xt[b % 2][:], xin[b * 512:(b + 1) * 512, :].rearrange("(j p) d -> p j d", p=128), writes=[txt[b % 2]])

        load_x(0)
        for b in range(nblk):
            bi = b % 2
            if b + 1 < nblk:
                load_x(b + 1)
            S.op("dve", lambda e: e.tensor_copy(out=xb[:], in_=xt[bi][:]), reads=[txt[bi]], writes=[txb])
            transpose_block(k, xb, txb, xT[bi], txT[bi], ident[:], tid, psT, tpsT, 512, ctr)
            cols = slice(b * 512, (b + 1) * 512)
            for gi, grp in enumerate([] if 'f' in PSKIP else FM_GROUPS):
                for m in grp:
                    pi = pctr[0] % 5
                    pctr[0] += 1
                    for kc in range(8):
                        S.op("pe", lambda e, m=m, kc=kc, pi=pi: e.matmul(psm[pi][:], lhsT=wsb[:, kc, m * 128:(m + 1) * 128], rhs=xT[bi][:, kc, :], start=(kc == 0), stop=(kc == 7)),
                             reads=[tw[kc], txT[bi][kc]], writes=[tpsm[pi]])
                    evac(stg[bi][:, m, :], psm[pi][:], [tpsm[pi]], [tstg[bi][gi]], scale=(0.125 if 16 <= m < 20 else None))
                S.dma("sp", k.fm[grp[0]:grp[-1] + 1, :, cols].rearrange("c p t -> p c t"), stg[bi][:, grp[0]:grp[-1] + 1, :], reads=[tstg[bi][gi]])
            for m in ([] if 'h' in PSKIP else range(4, 8)):
                pi = pctr[0] % 5
                pctr[0] += 1
                for kc in range(8):
                    S.op("pe", lambda e, m=m, kc=kc, pi=pi: e.matmul(psm[pi][:], lhsT=wsb[:, kc, m * 128:(m + 1) * 128], rhs=xT[bi][:, kc, :], start=(kc == 0), stop=(kc == 7)),
                         reads=[tw[kc], txT[bi][kc]], writes=[tpsm[pi]])
                evac(stf[bi][:, m - 4, :], psm[pi][:], [tpsm[pi]], [tstf[bi]])
            if 'h' not in PSKIP:
                S.dma("sp", k.fmf[:, :, cols].rearrange("c p t -> p c t"), stf[bi][:], reads=[tstf[bi]])
            for j in ([] if 'i' in PSKIP else range(4)):
                pi = pctr[0] % 5
                pctr[0] += 1
                for kc in range(8):
                    S.op("pe", lambda e, j=j, kc=kc, pi=pi: e.matmul(psm[pi][:], lhsT=xT[bi][:, kc, j * 128:(j + 1) * 128], rhs=wsb[:, kc, 1024:1536], start=(kc == 0), stop=(kc == 7)),
                         reads=[tw[kc], txT[bi][kc]], writes=[tpsm[pi]])
                evac(sti[bi][:, j, :], psm[pi][:], [tpsm[pi]], [tsti[bi]])
            if 'i' not in PSKIP:
                S.dma("pool", k.tmi[b * 512:(b + 1) * 512, :].rearrange("(j p) c -> p j c", p=128), sti[bi][:], reads=[tsti[bi]])
            for j in ([] if 'v' in PSKIP else range(4)):
                pi = pctr[0] % 5
                pctr[0] += 1
                for (c0, c1, o0) in ((2944, 3072, 0), (3200, 3328, 128), (3328, 3352, 256)):
                    if 'g' in PSKIP and o0 == 256:
                        continue
                    for kc in range(8):
                        S.op("pe", lambda e, j=j, kc=kc, pi=pi, c0=c0, c1=c1, o0=o0: e.matmul(psm[pi][:, o0:o0 + (c1 - c0)], lhsT=xT[bi][:, kc, j * 128:(j + 1) * 128], rhs=wsb[:, kc, c0:c1], start=(kc == 0), stop=(kc == 7)),
                             reads=[tw[kc], txT[bi][kc]], writes=[tpsm[pi]])
                evac(stv[bi][:, j, :], psm[pi][:, 0:256], [tpsm[pi]], [tstv[bi]], eng="dve")
                if 'G' not in PSKIP:
                    evac(stgt[bi][:, j, :], psm[pi][:, 256:280], [tpsm[pi]], [tstgt[bi]], eng="dve")
            if 'v' not in PSKIP:
              S.dma("pool", k.tmv[b * 512:(b + 1) * 512, :].rearrange("(j p) c -> p j c", p=128), stv[bi][:], reads=[tstv[bi]])
              if 'D' not in PSKIP:
                S.dma("sp" if 'Q' in PSKIP else "pool", k.tmg[b * 512:(b + 1) * 512, :].rearrange("(j p) c -> p j c", p=128), stgt[bi][:], reads=[tstgt[bi]])
        S.barrier()


def stage_hgrn(k, l):
    nc, S = k.nc, k.S
    with ExitStack() as es:
        cb = load_consts_bf(k, es, ["ident", "caus", "seg", "ones"])
        ident, tid = cb["ident"]
        caus, tcaus = cb["caus"]
        seg, tseg = cb["seg"]
        ones, tones = cb["ones"]
        lbt = _sb(es, nc, "h_lb", [128, 4], F32)
        tlb = T("lb")
        S.dma("sp", lbt[:], k.lbs[l].rearrange("(q p) -> p q", p=128), writes=[tlb], allow_slow_non_contiguous=True)
        oml = _sb(es, nc, "h_oml", [128, 4], F32)
        toml = T("oml")
        S.op("dve", lambda e: e.tensor_scalar(out=oml[:], in0=lbt[:], scalar1=-1.0, scalar2=1.0, op0=ALU.mult, op1=ALU.add), reads=[tlb], writes=[toml])
        gn = _sb(es, nc, "h_gn", [128, 1], F32)
        tgn = T("gn")
        S.dma("sp", gn[:], k.w["hg_norm_w"][l].rearrange("(p o) -> p o", o=1), writes=[tgn], allow_slow_non_contiguous=True)

        fT = _sb(es, nc, "h_f", [128, SEQ], F32); tf = T("f")
        qT = _sb(es, nc, "h_q", [128, SEQ], BF16); tq = T("q")
        gT = _sb(es, nc, "h_g", [128, SEQ], BF16); tg = T("g")
        vS = _sb(es, nc, "h_v", [64, 64, 128], BF16); tv = T("v")
        A = _sb(es, nc, "h_A", [128, SEQ], F32); tA = T("A")
        B = _sb(es, nc, "h_B", [128, SEQ], F32); tB = T("B")
        C = _sb(es, nc, "h_C", [128, SEQ], F32); tC = T("C")
        Dd = _sb(es, nc, "h_D", [128, SEQ], F32); tD = T("D")
        qt = _sb(es, nc, "h_qt", [128, SEQ], BF16); tqt = T("qt")
        kt_ = _sb(es, nc, "h_kt", [128, SEQ], BF16); tkt = T("kt")
        qb = _sb(es, nc, "h_qb", [128, SEQ], BF16); tqb = T("qb")
        kd = _sb(es, nc, "h_kd", [128, SEQ], BF16); tkd = T("kd")
        ebl = _sb(es, nc, "h_ebl", [128, 64], F32); tebl = T("ebl")
        AT = _sb(es, nc, "h_AT", [64, SEQ], BF16); tAT = [T("AT%d" % i) for i in range(8)]
        kdT = _sb(es, nc, "h_kdT", [64, 64, 128], BF16); tkdT = [T("kdT%d" % i) for i in range(8)]
        oT = _sb(es, nc, "h_oT", [128, SEQ], F32); toT = [T("oT%d" % i) for i in range(8)]
        St = _sb(es, nc, "h_S", [128, 128], F32); tSt = T("S")
        Sb = _sb(es, nc, "h_Sb", [128, 128], BF16); tSb = T("Sb")
        ostg = _sb(es, nc, "h_ostg", [128, SEQ], BF16); tostg = T("ostg")
        rstd = _sb(es, nc, "h_rstd", [128, 512], F32); trstd = T("rstd")
        psA = [_ps(es, nc, "h_psA%d" % i, [128, 512], F32) for i in range(2)]; tpsA = [T("psA%d" % i) for i in range(2)]
        psK = [_ps(es, nc, "h_psK%d" % i, [64, 1024], BF16) for i in range(2)]; tpsK = [T("psK%d" % i) for i in range(2)]
        psO = [_ps(es, nc, "h_psO%d" % i, [128, 512], F32) for i in range(2)]; tpsO = [T("psO%d" % i) for i in range(2)]
        psS = [_ps(es, nc, "h_psS%d" % i, [128, 512], F32) for i in range(2)]; tpsS = [T("psS%d" % i) for i in range(2)]

        def v3(t):
            return t[:].rearrange("p (c j) -> p c j", j=64)

        for s in range(k.nseq):
            for h in range(4):
                cols = slice(s * SEQ, (s + 1) * SEQ)
                S.dma("sp", fT[:], k.fmf[h, :, cols], writes=[tf])
                S.dma("sp", qT[:], k.fm[h, :, cols], writes=[tq])
                S.dma("sp", gT[:], k.fm[12 + h, :, cols], writes=[tg])
                S.dma("pool", vS[:], k.tmi[s * SEQ:(s + 1) * SEQ, h * 128:(h + 1) * 128].rearrange("(c j) v -> j c v", j=64), writes=[tv])
                S.op("act", lambda e: e.activation(out=A[:], in_=fT[:], func=AF.Sigmoid), reads=[tf], writes=[tA])
                S.op("dve", lambda e: e.tensor_scalar(out=A[:], in0=A[:], scalar1=oml[:, h:h + 1], scalar2=lbt[:, h:h + 1], op0=ALU.mult, op1=ALU.add), reads=[tA, toml, tlb], writes=[tA])
                S.op("act", lambda e: e.activation(out=B[:], in_=A[:], func=AF.Ln), reads=[tA], writes=[tB])
                S.op("dve", lambda e: e.tensor_tensor_scan(out=C[:], data0=seg[:], data1=B[:], initial=0.0, op0=ALU.mult, op1=ALU.add), reads=[tB, tseg], writes=[tC])
                S.op("dve", lambda e: e.tensor_scalar(out=A[:], in0=A[:], scalar1=-1.0, scalar2=1.0, op0=ALU.mult, op1=ALU.add), reads=[tA], writes=[tA])
                C3 = v3(C)
                S.op("act", lambda e: e.activation(out=B[:], in_=C[:], func=AF.Exp), reads=[tC], writes=[tB])
                S.op("dve", lambda e: e.tensor_tensor(out=qb[:], in0=B[:], in1=qT[:], op=ALU.mult), reads=[tB, tq], writes=[tqb])
                S.op("act", lambda e: e.activation(out=ebl[:], in_=C3[:, :, 63], func=AF.Exp), reads=[tC], writes=[tebl])
                S.op("dve", lambda e: e.tensor_tensor(out=v3(Dd), in0=C3, in1=C3[:, :, 31:32].to_broadcast([128, 64, 64]), op=ALU.subtract), reads=[tC], writes=[tD])
                S.op("act", lambda e: e.activation(out=B[:], in_=Dd[:], func=AF.Exp), reads=[tD], writes=[tB])
                S.op("dve", lambda e: e.tensor_tensor(out=qt[:], in0=B[:], in1=qT[:], op=ALU.mult), reads=[tB, tq], writes=[tqt])
                S.op("act", lambda e: e.activation(out=B[:], in_=Dd[:], func=AF.Exp, scale=-1.0), reads=[tD], writes=[tB])
                S.op("dve", lambda e: e.tensor_tensor(out=kt_[:], in0=B[:], in1=A[:], op=ALU.mult), reads=[tB, tA], writes=[tkt])
                S.op("dve", lambda e: e.tensor_tensor(out=v3(Dd), in0=C3[:, :, 63:64].to_broadcast([128, 64, 64]), in1=C3, op=ALU.subtract), reads=[tC], writes=[tD])
                S.op("act", lambda e: e.activation(out=B[:], in_=Dd[:], func=AF.Exp), reads=[tD], writes=[tB])
                S.op("dve", lambda e: e.tensor_tensor(out=kd[:], in0=B[:], in1=A[:], op=ALU.mult), reads=[tB, tA], writes=[tkd])
                for blk in range(8):
                    pi = blk % 2
                    for cc in range(8):
                        c = blk * 8 + cc
                        S.op("pe", lambda e, c=c, cc=cc, pi=pi: e.matmul(psA[pi][0:64, cc * 64:(cc + 1) * 64], lhsT=kt_[:, c * 64:(c + 1) * 64], rhs=qt[:, c * 64:(c + 1) * 64], start=True, stop=True),
                             reads=[tkt, tqt], writes=[tpsA[pi]])
                    S.op("dve", lambda e, blk=blk, pi=pi: e.tensor_tensor(out=AT[:, blk * 512:(blk + 1) * 512], in0=psA[pi][0:64, :], in1=caus[:], op=ALU.mult),
                         reads=[tpsA[pi], tcaus], writes=[tAT[blk]])
                    for cc in range(8):
                        c = blk * 8 + cc
                        S.op("pe", lambda e, c=c, cc=cc, pi=pi: e.transpose(out=psK[pi][:, cc * 128:(cc + 1) * 128], in_=kd[:, c * 64:(c + 1) * 64], identity=ident[:]),
                             reads=[tkd, tid], writes=[tpsK[pi]])
                    S.op("act", lambda e, blk=blk, pi=pi: e.copy(out=kdT[:, blk * 8:(blk + 1) * 8, :], in_=psK[pi][:].rearrange("p (c d) -> p c d", d=128)),
                         reads=[tpsK[pi]], writes=[tkdT[blk]])
                for c in range(64):
                    blk, cc = c // 8, c % 8
                    pi = blk % 2
                    S.op("pe", lambda e, c=c, cc=cc, pi=pi: e.matmul(psO[pi][:, cc * 64:(cc + 1) * 64], lhsT=vS[:, c, :], rhs=AT[:, c * 64:(c + 1) * 64], start=True, stop=(c == 0)),
                         reads=[tv, tAT[blk]], writes=[tpsO[pi]])
                    if c > 0:
                        S.op("pe", lambda e, c=c, cc=cc, pi=pi: e.matmul(psO[pi][:, cc * 64:(cc + 1) * 64], lhsT=Sb[:], rhs=qb[:, c * 64:(c + 1) * 64], start=False, stop=True),
                             reads=[tSb, tqb], writes=[tpsO[pi]])
                    if cc == 7:
                        S.op("act", lambda e, blk=blk, pi=pi: e.copy(out=oT[:, blk * 512:(blk + 1) * 512], in_=psO[pi][:]), reads=[tpsO[pi]], writes=[toT[blk]])
                    if c < 63:
                        si = c % 2
                        S.op("pe", lambda e, c=c, si=si: e.matmul(psS[si][:, 0:128], lhsT=kdT[:, c, :], rhs=vS[:, c, :], start=True, stop=True),
                             reads=[tkdT[blk], tv], writes=[tpsS[si]])
                        if c == 0:
                            S.op("dve", lambda e, si=si: e.tensor_copy(out=St[:], in_=psS[si][:, 0:128]), reads=[tpsS[si]], writes=[tSt])
                        else:
                            S.op("dve", lambda e, c=c, si=si: e.scalar_tensor_tensor(out=St[:], in0=St[:], scalar=ebl[:, c:c + 1], in1=psS[si][:, 0:128], op0=ALU.mult, op1=ALU.add),
                                 reads=[tSt, tebl, tpsS[si]], writes=[tSt])
                        S.op("act", lambda e: e.copy(out=Sb[:], in_=St[:]), reads=[tSt], writes=[tSb])
                S.op("act", lambda e: e.activation(out=qt[:], in_=oT[:], func=AF.Square), reads=toT, writes=[tqt])
                S.op("act", lambda e: e.activation(out=kd[:], in_=gT[:], func=AF.Silu), reads=[tg], writes=[tkd])
                for blk in range(8):
                    pi = blk % 2
                    bs = slice(blk * 512, (blk + 1) * 512)
                    S.op("pe", lambda e, bs=bs, pi=pi: e.matmul(psA[pi][:], lhsT=ones[:], rhs=qt[:, bs], start=True, stop=True), reads=[tones, tqt], writes=[tpsA[pi]])
                    S.op("dve", lambda e, pi=pi: e.tensor_scalar(out=rstd[:], in0=psA[pi][:], scalar1=1.0 / 128.0, scalar2=1e-6, op0=ALU.mult, op1=ALU.add), reads=[tpsA[pi]], writes=[trstd])
                    S.op("act", lambda e: e.activation(out=rstd[:], in_=rstd[:], func=AF.Sqrt), reads=[trstd], writes=[trstd])
                    S.op("dve", lambda e: e.reciprocal(out=rstd[:], in_=rstd[:]), reads=[trstd], writes=[trstd])
                    S.op("dve", lambda e, bs=bs: e.tensor_tensor(out=rstd[:], in0=rstd[:], in1=oT[:, bs], op=ALU.mult), reads=[trstd, toT[blk]], writes=[trstd])
                    S.op("dve", lambda e, bs=bs: e.scalar_tensor_tensor(out=ostg[:, bs], in0=rstd[:], scalar=gn[:, 0:1], in1=kd[:, bs], op0=ALU.mult, op1=ALU.mult),
                         reads=[trstd, tgn, tkd], writes=[tostg])
                S.dma("sp", k.catT[h, :, cols], ostg[:], reads=[tostg])
        S.barrier()


def stage_nsa(k, l):
    nc, S = k.nc, k.S
    with ExitStack() as es:
        cb = load_consts_bf(k, es, ["ident", "agg"])
        ident, tid = cb["ident"]
        esel = _sb(es, nc, "n_esel2", [128, 32, 128], BF16); tesel = T("esel2")
        for half in range(2):
            S.dma("pool", esel[half * 64:(half + 1) * 64, :, :], k.c["esel"], writes=[tesel])
        aggb, tagg = cb["agg"]
        idf = _sb(es, nc, "n_idf", [128, 128], F32); tidf = T("idf")
        S.dma("sp", idf[:], k.c["ident"], writes=[tidf])
        selb = _sb(es, nc, "n_selb", [128, 32, 64], F32); tselb = T("selb")
        S.dma("sp", selb[:], k.c["selb"].rearrange("(c p) j -> p c j", p=128), writes=[tselb])
        w1 = {}
        w2 = {}
        pos = {}
        hb = {}
        for kv in "kv":
            w1[kv] = (_sb(es, nc, "n_w1" + kv, [128, 32, 128], BF16), T("w1" + kv))
            for half in range(2):
                S.dma("pool", w1[kv][0][half * 64:(half + 1) * 64, :, :], k.w["cmp_w1_" + kv][l].rearrange("(p d) h -> d p h", d=64), writes=[w1[kv][1]])
            pos[kv] = (_sb(es, nc, "n_pos" + kv, [64, 32], BF16), T("pos" + kv))
            S.dma("pool", pos[kv][0][:], k.w["cmp_pos_" + kv][l].rearrange("p d -> d p"), writes=[pos[kv][1]], allow_slow_non_contiguous=True)
            hb[kv] = (_sb(es, nc, "n_hb" + kv, [128, 1], F32), T("hb" + kv))
        w2[("k")] = (_sb(es, nc, "n_w2k", [128, 128], BF16), T("w2k"))
        for half in range(2):
            S.dma("pool", w2["k"][0][:, half * 64:(half + 1) * 64], k.w["cmp_w2_k"][l], writes=[w2["k"][1]])
        w2["v"] = (_sb(es, nc, "n_w2v", [128, 64], BF16), T("w2v"))
        S.dma("pool", w2["v"][0][:], k.w["cmp_w2_v"][l], writes=[w2["v"][1]])

        qT = [_sb(es, nc, "n_q%d" % i, [128, SEQ], BF16) for i in range(2)]; tq = [T("q%d" % i) for i in range(2)]
        ksT = _sb(es, nc, "n_ks", [128, SEQ], BF16); tks = T("ks")
        kwT = _sb(es, nc, "n_kw", [128, SEQ], BF16); tkw = T("kw")
        srcT = {kv: (_sb(es, nc, "n_src" + kv, [128, SEQ], BF16), T("src" + kv)) for kv in "kv"}
        vs = _sb(es, nc, "n_vs", [128, 32, 128], BF16); tvs = T("vs")
        vw = _sb(es, nc, "n_vw", [128, 32, 128], BF16); tvw = T("vw")
        kcT = _sb(es, nc, "n_kcT", [128, 256], BF16); tkcT = T("kcT")
        vca = _sb(es, nc, "n_vca", [128, 2, 128], BF16); tvca = T("vca")
        gt = _sb(es, nc, "n_gt", [128, 32, 24], F32); tgt = T("gt")
        tabS = _sb(es, nc, "n_tS", [128, 4, WS], BF16); ttS = T("tS")
        tabW = _sb(es, nc, "n_tW", [128, 4, WW], BF16); ttW = T("tW")
        tabC = _sb(es, nc, "n_tC", [128, 4, WC], BF16); ttC = T("tC")
        gu = _sb(es, nc, "n_gu", [128, 256], F32); tgu = T("gu")
        gu2 = _sb(es, nc, "n_gu2", [128, 256], F32); tgu2 = T("gu2")
        ge = _sb(es, nc, "n_ge", [128, 256], BF16); tge = T("ge")
        nselT = _sb(es, nc, "n_nselT", [128, 512], BF16); tnselT = T("nselT")
        Eb = [_sb(es, nc, "n_E%d" % i, [128, 512], BF16) for i in range(4)]; tE = [T("E%d" % i) for i in range(4)]
        Ex = [_sb(es, nc, "n_Ex%d" % i, [128, 512], BF16) for i in range(4)]; tEx = [T("Ex%d" % i) for i in range(4)]
        oacc = [_sb(es, nc, "n_oacc%d" % i, [128, 4, 4, 64], F32) for i in range(2)]; toacc = [T("oacc0"), T("oacc1")]
        imp = _sb(es, nc, "n_imp", [128, 4, 64], F32); timp = T("imp")
        rc4 = _sb(es, nc, "n_rc4", [128, 4], F32); trc4 = T("rc4")
        cf4 = _sb(es, nc, "n_cf4", [128, 4], F32); tcf4 = T("cf4")
        tmp4 = _sb(es, nc, "n_tmp4", [128, 4, 64], F32); ttmp4 = T("tmp4")
        sc = _sb(es, nc, "n_sc", [128, 64], F32); tsc = T("sc")
        sc2 = _sb(es, nc, "n_sc2", [128, 64], F32); tsc2 = T("sc2")
        m8 = _sb(es, nc, "n_m8", [128, 8], F32); tm8 = T("m8")
        nsel = _sb(es, nc, "n_nsel", [128, 4, 128], F32); tnsel = T("nsel")
        ostg = _sb(es, nc, "n_ostg", [128, 2, 512], BF16); tostg = T("ostg")

        psS = [_ps(es, nc, "n_psS%d" % i, [128, 512], F32) for i in range(3)]; tpsS = [T("psS%d" % i) for i in range(3)]
        psCA = _ps(es, nc, "n_psCA", [128, 512], F32); tpsCA = T("psCA")
        psCB = _ps(es, nc, "n_psCB", [128, 512], F32); tpsCB = T("psCB")
        psOS = _ps(es, nc, "n_psOS", [128, 512], F32); tpsOS = T("psOS")
        psOW = _ps(es, nc, "n_psOW", [128, 512], F32); tpsOW = T("psOW")
        psT = _ps(es, nc, "n_psT", [128, 512], F32); tpsT = T("psT")
        psX = psT; tpsX = tpsT
        sctr = [0]
        ectr = [0]

        S.op("dve", lambda e: e.memset(vs[:], 1.0), writes=[tvs])
        S.op("dve", lambda e: e.memset(vw[:], 1.0), writes=[tvw])

        def score_tile(kT_ap, kreads, q_ap, qreads, tab_ap, tabreads, extra=None, cr=(0, 512)):
            c0, c1 = cr
            pi = sctr[0] % 3
            sctr[0] += 1
            S.op("pe", lambda e: e.matmul(psS[pi][:, c0:c1], lhsT=kT_ap, rhs=q_ap[:, c0:c1], start=True, stop=(extra is None)), reads=kreads + qreads, writes=[tpsS[pi]])
            if extra is not None:
                S.op("pe", lambda e: e.matmul(psS[pi][:, c0:c1], lhsT=extra[0], rhs=extra[1][:, c0:c1], start=False, stop=True), reads=extra[2], writes=[tpsS[pi]])
            ei = ectr[0] % 4
            ectr[0] += 1
            S.op("act", lambda e: e.activation(out=Ex[ei][:, c0:c1], in_=psS[pi][:, c0:c1], func=AF.Exp), reads=[tpsS[pi]], writes=[tEx[ei]])
            meng = "dve"
            S.op(meng, lambda e: e.tensor_tensor(out=Eb[ei][:, c0:c1], in0=Ex[ei][:, c0:c1], in1=tab_ap[:, c0:c1], op=ALU.mult), reads=[tEx[ei]] + tabreads, writes=[tE[ei]])
            return ei

        for s in range(k.nseq):
            cols = slice(s * SEQ, (s + 1) * SEQ)
            S.dma("sp", gt[:], k.tmg[s * SEQ:(s + 1) * SEQ, :].rearrange("(c p) g -> p c g", p=128), writes=[tgt])
            S.op("act", lambda e: e.activation(out=gt[:], in_=gt[:], func=AF.Sigmoid), reads=[tgt], writes=[tgt])
            for g in range(2):
                for i in range(2):
                    S.dma("sp", qT[i][:], k.fm[16 + 2 * g + i, :, cols], writes=[tq[i]])
                for half in range(2):
                    S.dma("sp", ksT[half * 64:(half + 1) * 64, :], k.fm[22, g * 64:(g + 1) * 64, cols], writes=[tks])
                    S.dma("sp", kwT[half * 64:(half + 1) * 64, :], k.fm[24, g * 64:(g + 1) * 64, cols], writes=[tkw])
                S.dma("sp", srcT["k"][0][0:64, :], k.fm[20, g * 64:(g + 1) * 64, cols], writes=[srcT["k"][1]])
                S.dma("sp", srcT["v"][0][0:64, :], k.fm[21, g * 64:(g + 1) * 64, cols], writes=[srcT["v"][1]])
                S.dma("pool", vs[:, :, 0:64], k.tmv[s * SEQ:(s + 1) * SEQ, g * 64:(g + 1) * 64].rearrange("(c p) d -> p c d", p=128), writes=[tvs])
                S.dma("pool", vw[:, :, 0:64], k.tmv[s * SEQ:(s + 1) * SEQ, 128 + g * 64:128 + (g + 1) * 64].rearrange("(c p) d -> p c d", p=128), writes=[tvw])
                S.dma("pool", tabS[:], k.tS[4 * g:4 * g + 4].rearrange("h p w -> p h w"), writes=[ttS])
                S.dma("pool", tabW[:], k.tW[4 * g:4 * g + 4].rearrange("h p w -> p h w"), writes=[ttW])
                S.dma("pool", tabC[:], k.tC[4 * g:4 * g + 4].rearrange("h p w -> p h w"), writes=[ttC])
                for kv in ("" if 'c' in NSKIP else "kv"):
                    src, tsrc = srcT[kv]
                    src3 = src[0:64, :].rearrange("q (n s) -> q n s", s=16)
                    w1t, tw1 = w1[kv]
                    for p in range(32):
                        S.op("pe", lambda e, p=p: e.matmul(psX[:, 300:301], lhsT=w1t[0:64, p, :], rhs=pos[kv][0][:, p:p + 1], start=(p == 0), stop=(p == 31)),
                             reads=[tw1, pos[kv][1]], writes=[tpsX])
                    S.op("dve", lambda e: e.tensor_copy(out=hb[kv][0][:], in_=psX[:, 300:301]), reads=[tpsX], writes=[hb[kv][1]])
                    for p in range(32):
                        S.op("pe", lambda e, p=p: e.matmul(psX[:, 0:255], lhsT=w1t[0:64, p, :], rhs=src3[:, (p // 16):(p // 16) + 255, p % 16], start=(p == 0), stop=(p == 31)),
                             reads=[tw1, tsrc], writes=[tpsX])
                    S.op("dve", lambda e: e.memset(ge[:], 0.0), writes=[tge])
                    S.op("act", lambda e: e.activation(out=gu[:, 0:255], in_=psX[:, 0:255], func=AF.Identity, bias=hb[kv][0][:, 0:1]), reads=[tpsX, hb[kv][1]], writes=[tgu])
                    S.op("dve", lambda e: e.tensor_tensor(out=gu2[:, 0:255], in0=gu[:, 0:255], in1=gu[:, 0:255], op=ALU.mult), reads=[tgu], writes=[tgu2])
                    S.op("dve", lambda e: e.tensor_scalar(out=gu2[:, 0:255], in0=gu2[:, 0:255], scalar1=0.044715, scalar2=1.0, op0=ALU.mult, op1=ALU.add), reads=[tgu2], writes=[tgu2])
                    S.op("dve", lambda e: e.tensor_tensor(out=gu2[:, 0:255], in0=gu2[:, 0:255], in1=gu[:, 0:255], op=ALU.mult), reads=[tgu2, tgu], writes=[tgu2])
                    S.op("act", lambda e: e.activation(out=gu2[:, 0:255], in_=gu2[:, 0:255], func=AF.Sigmoid, scale=2.0 * 0.7978845608028654), reads=[tgu2], writes=[tgu2])
                    S.op("dve", lambda e: e.tensor_tensor(out=ge[:, 0:255], in0=gu2[:, 0:255], in1=gu[:, 0:255], op=ALU.mult), reads=[tgu2, tgu], writes=[tge])
                    if kv == "k":
                        S.op("pe", lambda e: e.matmul(psX[:, 0:256], lhsT=w2["k"][0][:], rhs=ge[:], start=True, stop=True), reads=[w2["k"][1], tge], writes=[tpsX])
                        S.op("act", lambda e: e.copy(out=kcT[:], in_=psX[:, 0:256]), reads=[tpsX], writes=[tkcT])
                    else:
                        S.op("dve", lambda e: e.memset(vca[:], 1.0), writes=[tvca])
                        for kt in range(2):
                            S.op("pe", lambda e, kt=kt: e.matmul(psX[:, kt * 64:(kt + 1) * 64], lhsT=ge[:, kt * 128:(kt + 1) * 128], rhs=w2["v"][0][:], start=True, stop=True),
                                 reads=[w2["v"][1], tge], writes=[tpsX])
                        S.op("act", lambda e: e.copy(out=vca[:, :, 0:64], in_=psX[:, 0:128].rearrange("p (a b) -> p a b", b=64)), reads=[tpsX], writes=[tvca])
                pipe = []
                b1_ops = []

                def submit(score_args, pv_fn, post_fn=None, extra=None, cr=(0, 512)):
                    ei = score_tile(*score_args, extra=extra, cr=cr)
                    if b1_ops:
                        b1_ops.pop(0)()
                    if len(pipe) >= 2:
                        ppv, ppost = pipe.pop(0)
                        ppv()
                        if ppost is not None:
                            ppost()
                    pipe.append((lambda ei=ei: pv_fn(ei), post_fn))

                def drain():
                    while pipe:
                        ppv, ppost = pipe.pop(0)
                        ppv()
                        if ppost is not None:
                            ppost()

                accs = [(psOS, tpsOS), (psOW, tpsOW)]

                def phase_A(qb):
                    qs = slice(qb * 512, (qb + 1) * 512)
                    oa, toa = oacc[qb % 2], toacc[qb % 2]
                    for h in range(4):
                        hp = (h % 2) * 64
                        q_ap = qT[h // 2][hp:hp + 64, qs]
                        kts = [0] if qb < 4 else [0, 1]
                        if h % 2 == 0:
                            pa, tpa, pb, tpb = psCA, tpsCA, psCB, tpsCB
                        else:
                            pa, tpa, pb, tpb = psOS, tpsOS, psOW, tpsOW

                        def pv(ei, kt, pa=pa, tpa=tpa, pb=pb, tpb=tpb, kts=kts):
                            for tc in range(4):
                                S.op("pe", lambda e: e.matmul(pa[:, tc * 65:(tc + 1) * 65], lhsT=Eb[ei][:, tc * 128:(tc + 1) * 128], rhs=vca[:, kt, 0:65], start=(kt == 0 and tc == 0), stop=(kt == kts[-1]), skip_group_check=True),
                                     reads=[tE[ei], tvca], writes=[tpa])
                            for tc in range(4):
                                S.op("pe", lambda e: e.matmul(pb[:, tc * 64:(tc + 1) * 64], lhsT=Eb[ei][:, tc * 128:(tc + 1) * 128], rhs=aggb[:, kt, :], start=(kt == 0 and tc == 0), stop=(kt == kts[-1]), skip_group_check=True),
                                     reads=[tE[ei], tagg], writes=[tpb])

                        def post(h=h, pa=pa, tpa=tpa, pb=pb, tpb=tpb):
                            cav = pa[:, 0:260].rearrange("p (a b) -> p a b", b=65)
                            S.op("dve", lambda e: e.tensor_scalar(out=rc4[:], in0=cav[:, :, 64], scalar1=1e-30, scalar2=None, op0=ALU.add), reads=[tpa], writes=[trc4])
                            S.op("dve", lambda e: e.reciprocal(out=rc4[:], in_=rc4[:]), reads=[trc4], writes=[trc4])
                            S.op("dve", lambda e: e.tensor_tensor(out=cf4[:], in0=rc4[:], in1=gt[:, qb * 4:(qb + 1) * 4, 0 * 8 + g * 4 + h], op=ALU.mult), reads=[trc4, tgt], writes=[tcf4])
                            S.op("dve", lambda e: e.tensor_tensor(out=oa[:, :, h, :], in0=cav[:, :, 0:64], in1=cf4[:].unsqueeze(2).to_broadcast([128, 4, 64]), op=ALU.mult),
                                 reads=[tpa, tcf4], writes=[toa])
                            cbv = pb[:, 0:256].rearrange("p (a b) -> p a b", b=64)
                            if h == 0:
                                S.op("dve", lambda e: e.tensor_tensor(out=imp[:], in0=cbv, in1=rc4[:].unsqueeze(2).to_broadcast([128, 4, 64]), op=ALU.mult), reads=[tpb, trc4], writes=[timp])
                            else:
                                S.op("dve", lambda e: e.tensor_tensor(out=tmp4[:], in0=cbv, in1=rc4[:].unsqueeze(2).to_broadcast([128, 4, 64]), op=ALU.mult), reads=[tpb, trc4], writes=[ttmp4])
                                S.op("dve", lambda e: e.tensor_tensor(out=imp[:], in0=imp[:], in1=tmp4[:], op=ALU.add), reads=[timp, ttmp4], writes=[timp])

                        for kt in kts:
                            off = min(512 * qb - 2048 * kt, 2560)
                            submit((kcT[hp:hp + 64, kt * 128:(kt + 1) * 128], [tkcT], q_ap, [tq[h // 2]], tabC[:, h, off:off + 512], [ttC]),
                                   lambda ei, kt=kt, pv=pv: pv(ei, kt), post if kt == kts[-1] else None)

                def phase_B1(qb):
                    for tc in range(4):
                        T4 = qb * 4 + tc
                        b1_ops.append(lambda tc=tc, T4=T4: S.op("dve", lambda e: e.tensor_tensor(out=sc[:], in0=imp[:, tc, :], in1=selb[:, T4, :], op=ALU.add), reads=[timp, tselb], writes=[tsc]))
                        b1_ops.append(lambda: S.op("dve", lambda e: e.max(out=m8[:], in_=sc[:]), reads=[tsc], writes=[tm8]))
                        b1_ops.append(lambda: S.op("dve", lambda e: e.match_replace(out=sc2[:], in_to_replace=m8[:], in_values=sc[:], imm_value=-2e9), reads=[tsc, tm8], writes=[tsc2]))
                        b1_ops.append(lambda: S.op("dve", lambda e: e.max(out=m8[:], in_=sc2[:]), reads=[tsc2], writes=[tm8]))
                        b1_ops.append(lambda tc=tc: S.op("dve", lambda e: e.tensor_scalar(out=nsel[:, tc, 0:64], in0=sc[:], scalar1=m8[:, 7:8], scalar2=NEGM, op0=ALU.is_lt, op1=ALU.mult), reads=[tsc, tm8], writes=[tnsel]))
                        b1_ops.append(lambda tc=tc: S.op("dve", lambda e: e.tensor_scalar(out=nsel[:, tc, 64:128], in0=sc[:], scalar1=m8[:, 7:8], scalar2=NEGM, op0=ALU.is_lt, op1=ALU.mult), reads=[tsc, tm8], writes=[tnsel]))

                def phase_B2(qb):
                    for tc in range(4):
                        S.op("pe", lambda e: e.transpose(out=psT[:, tc * 128:(tc + 1) * 128], in_=nsel[:, tc, :], identity=idf[:]), reads=[tnsel, tidf], writes=[tpsT])
                    S.op("act", lambda e: e.copy(out=nselT[:], in_=psT[:]), reads=[tpsT], writes=[tnselT])

                def combine_post(qb, h, pso, tpso, br):
                    oa, toa = oacc[qb % 2], toacc[qb % 2]

                    def post():
                        ov = pso[:, 0:260].rearrange("p (a b) -> p a b", b=65)
                        S.op("dve", lambda e: e.reciprocal(out=rc4[:], in_=ov[:, :, 64]), reads=[tpso], writes=[trc4])
                        S.op("dve", lambda e: e.tensor_tensor(out=cf4[:], in0=rc4[:], in1=gt[:, qb * 4:(qb + 1) * 4, br * 8 + g * 4 + h], op=ALU.mult), reads=[trc4, tgt], writes=[tcf4])
                        S.op("dve", lambda e: e.tensor_tensor(out=tmp4[:], in0=ov[:, :, 0:64], in1=cf4[:].unsqueeze(2).to_broadcast([128, 4, 64]), op=ALU.mult), reads=[tpso, tcf4], writes=[ttmp4])
                        S.op("pool", lambda e: e.tensor_tensor(out=oa[:, :, h, :], in0=oa[:, :, h, :], in1=tmp4[:], op=ALU.add), reads=[toa, ttmp4], writes=[toa])
                    return post

                def phase_W(qb):
                    qs = slice(qb * 512, (qb + 1) * 512)
                    for h in range(4):
                        hp = (h % 2) * 64
                        q_ap = qT[h // 2][hp:hp + 64, qs]
                        pso, tpso = accs[h % 2]
                        kts = list(range(max(0, 4 * qb - 4), 4 * qb + 4))

                        def pv(ei, kt, pso=pso, tpso=tpso, kts=kts):
                            for tc in range(4):
                                T4 = 4 * qb + tc
                                lo = max(0, T4 - 4)
                                if kt > T4 or kt < lo:
                                    continue
                                S.op("pe", lambda e: e.matmul(pso[:, tc * 65:(tc + 1) * 65], lhsT=Eb[ei][:, tc * 128:(tc + 1) * 128], rhs=vw[:, kt, 0:65], start=(kt == kts[0] and tc == 0), stop=(kt == T4), skip_group_check=True),
                                     reads=[tE[ei], tvw], writes=[tpso])

                        for kt in kts:
                            off = 512 * qb - 128 * kt + 384
                            submit((kwT[hp:hp + 64, kt * 128:(kt + 1) * 128], [tkw], q_ap, [tq[h // 2]], tabW[:, h, off:off + 512], [ttW]),
                                   lambda ei, kt=kt, pv=pv: pv(ei, kt), combine_post(qb, h, pso, tpso, 2) if kt == kts[-1] else None,
                                   cr=(max(0, kt - 4 * qb) * 128, (min(3, kt + 4 - 4 * qb) + 1) * 128))

                def phase_S(qb):
                    qs = slice(qb * 512, (qb + 1) * 512)
                    for h in range(4):
                        hp = (h % 2) * 64
                        q_ap = qT[h // 2][hp:hp + 64, qs]
                        pso, tpso = accs[h % 2]
                        kts = list(range(4 * qb + 4))

                        def pv(ei, kt, pso=pso, tpso=tpso):
                            for tc in range(4):
                                T4 = 4 * qb + tc
                                if kt > T4:
                                    continue
                                S.op("pe", lambda e: e.matmul(pso[:, tc * 65:(tc + 1) * 65], lhsT=Eb[ei][:, tc * 128:(tc + 1) * 128], rhs=vs[:, kt, 0:65], start=(kt == 0 and tc == 0), stop=(kt == T4), skip_group_check=True),
                                     reads=[tE[ei], tvs], writes=[tpso])

                        for kt in kts:
                            off = min(512 * qb - 128 * kt, 256) + 384
                            submit((ksT[hp:hp + 64, kt * 128:(kt + 1) * 128], [tks], q_ap, [tq[h // 2]], tabS[:, h, off:off + 512], [ttS]),
                                   lambda ei, kt=kt, pv=pv: pv(ei, kt), combine_post(qb, h, pso, tpso, 1) if kt == kts[-1] else None,
                                   extra=(esel[hp:hp + 64, kt, :], nselT[hp:hp + 64, :], [tesel, tnselT]), cr=(max(0, kt - 4 * qb) * 128, 512))

                def phase_F(qb):
                    oa, toa = oacc[qb % 2], toacc[qb % 2]
                    for c2 in range(2):
                        for tc in range(4):
                            S.op("pe", lambda e: e.transpose(out=psT[:, tc * 128:(tc + 1) * 128], in_=oa[:, tc, 2 * c2:2 * c2 + 2, :].rearrange("p a b -> p (a b)"), identity=idf[:]),
                                 reads=[toa, tidf], writes=[tpsT])
                        S.op("act", lambda e: e.copy(out=ostg[:, c2, :], in_=psT[:]), reads=[tpsT], writes=[tostg])
                    S.dma("sp", k.catT[4 + 2 * g:6 + 2 * g, :, s * SEQ + qb * 512:s * SEQ + (qb + 1) * 512].rearrange("c p t -> p c t"), ostg[:], reads=[tostg])

                for qb in range(8):
                    phase_A(qb)
                    drain()
                    if qb > 0:
                        phase_F(qb - 1)
                    phase_B1(qb)
                    phase_W(qb)
                    while b1_ops:
                        b1_ops.pop(0)()
                    phase_B2(qb)
                    phase_S(qb)
                drain()
                phase_F(7)
        S.barrier()


def layer_norm_rows(k, y, ty, dst, tdst, gam, tgam, bet, tbet, st, tst, mv, tmv):
    S = k.S
    for q in range(2):
        S.op("dve", lambda e, q=q: e.bn_stats(out=st[:, q, :], in_=y[:, q * 512:(q + 1) * 512]), reads=[ty], writes=[tst])
    S.op("dve", lambda e: e.bn_aggr(out=mv[:, 0:2], in_=st[:].rearrange("p a b -> p (a b)")), reads=[tst], writes=[tmv])
    S.op("dve", lambda e: e.tensor_scalar(out=mv[:, 2:3], in0=mv[:, 1:2], scalar1=1e-5, scalar2=None, op0=ALU.add), reads=[tmv], writes=[tmv])
    S.op("act", lambda e: e.activation(out=mv[:, 2:3], in_=mv[:, 2:3], func=AF.Sqrt), reads=[tmv], writes=[tmv])
    S.op("dve", lambda e: e.reciprocal(out=mv[:, 2:3], in_=mv[:, 2:3]), reads=[tmv], writes=[tmv])
    S.op("dve", lambda e: e.tensor_scalar(out=y, in0=y, scalar1=mv[:, 0:1], scalar2=mv[:, 2:3], op0=ALU.subtract, op1=ALU.mult), reads=[ty, tmv], writes=[ty])
    S.op("pool", lambda e: e.tensor_tensor(out=y, in0=y, in1=gam, op=ALU.mult), reads=[ty, tgam], writes=[ty])
    S.op("pool", lambda e: e.tensor_tensor(out=dst, in0=y, in1=bet, op=ALU.add), reads=[ty, tbet], writes=[tdst])


def load_bcast(k, es, name, src_row):
    nc, S = k.nc, k.S
    t = _sb(es, nc, name, [128, D], F32)
    tt = T(name)
    S.dma("sp", t[:], src_row.rearrange("(o d) -> o d", o=1).to_broadcast([128, D]), writes=[tt])
    return t, tt


def stage_out(k, l, xin):
    nc, S = k.nc, k.S
    NT = k.NT
    with ExitStack() as es:
        wsb = _sb(es, nc, "o_w", [128, 8, D], BF16)
        tw = [T("w%d" % i) for i in range(8)]
        for kc in range(8):
            S.dma("pool", wsb[:, kc, :], k.w["w_out"][l, kc * 128:(kc + 1) * 128, :], writes=[tw[kc]])
        gam, tgam = load_bcast(k, es, "o_gam", k.w["ln1_g"][l])
        bet, tbet = load_bcast(k, es, "o_bet", k.w["ln1_b"][l])
        cT = [_sb(es, nc, "o_cT%d" % i, [128, 8, 512], BF16) for i in range(2)]; tcT = [T("cT%d" % i) for i in range(2)]
        xt = [_sb(es, nc, "o_xt%d" % i, [128, 4, D], F32) for i in range(2)]; txt = [T("xt%d" % i) for i in range(2)]
        y = [_sb(es, nc, "o_y%d" % i, [128, D], F32) for i in range(2)]; ty = [T("y%d" % i) for i in range(2)]
        yo = [_sb(es, nc, "o_yo%d" % i, [128, 4, D], F32) for i in range(2)]; tyo = [T("yo%d" % i) for i in range(2)]
        st = _sb(es, nc, "o_st", [128, 2, 6], F32); tst = T("st")
        mv = _sb(es, nc, "o_mv", [128, 4], F32); tmv = T("mv")
        ps = [_ps(es, nc, "o_ps%d" % i, [128, 512], F32) for i in range(4)]; tps = [T("ps%d" % i) for i in range(4)]
        nblk = NT // 512

        def load(b):
            bi = b % 2
            S.dma("sp", cT[bi][:], k.catT[:, :, b * 512:(b + 1) * 512].rearrange("c p t -> p c t"), writes=[tcT[bi]])
            S.dma("sp", xt[bi][:], xin[b * 512:(b + 1) * 512, :].rearrange("(j p) d -> p j d", p=128), writes=[txt[bi]])

        load(0)
        pc = 0
        for b in range(nblk):
            bi = b % 2
            if b + 1 < nblk:
                load(b + 1)
            for j in range(4):
                yi = j % 2
                for half in range(2):
                    pi = pc % 4
                    pc += 1
                    for kc in range(8):
                        S.op("pe", lambda e, j=j, kc=kc, pi=pi, half=half: e.matmul(ps[pi][:], lhsT=cT[bi][:, kc, j * 128:(j + 1) * 128], rhs=wsb[:, kc, half * 512:(half + 1) * 512], start=(kc == 0), stop=(kc == 7)),
                             reads=[tcT[bi], tw[kc]], writes=[tps[pi]])
                    S.op("dve", lambda e, j=j, pi=pi, half=half, yi=yi: e.scalar_tensor_tensor(out=y[yi][:, half * 512:(half + 1) * 512], in0=xt[bi][:, j, half * 512:(half + 1) * 512], scalar=ALPHA, in1=ps[pi][:], op0=ALU.mult, op1=ALU.add),
                         reads=[txt[bi], tps[pi]], writes=[ty[yi]])
                layer_norm_rows(k, y[yi][:], ty[yi], yo[bi][:, j, :], tyo[bi], gam[:], tgam, bet[:], tbet, st, tst, mv, tmv)
            S.dma("sp", k.x1[b * 512:(b + 1) * 512, :].rearrange("(j p) d -> p j d", p=128), yo[bi][:], reads=[tyo[bi]])
        S.barrier()


def stage_ffn(k, l, xout):
    nc, S = k.nc, k.S
    NT = k.NT
    NB = 256
    nj = NB // 128
    NF = D_FF // 128
    with ExitStack() as es:
        cb = load_consts_bf(k, es, ["ident"])
        ident, tid = cb["ident"]
        wg = _sb(es, nc, "f_wg", [128, 8, D_FF], BF16); twg = [T("wg%d" % i) for i in range(8)]
        wu = _sb(es, nc, "f_wu", [128, 8, D_FF], BF16); twu = [T("wu%d" % i) for i in range(8)]
        wd = _sb(es, nc, "f_wd", [128, NF, D], BF16); twd = [T("wd%d" % i) for i in range(NF)]
        for kc in range(8):
            S.dma("pool", wg[:, kc, :], k.w["w_ffn_gate"][l, kc * 128:(kc + 1) * 128, :], writes=[twg[kc]])
            S.dma("pool", wu[:, kc, :], k.w["w_ffn_up"][l, kc * 128:(kc + 1) * 128, :], writes=[twu[kc]])
        for f in range(NF):
            S.dma("pool", wd[:, f, :], k.w["w_ffn_down"][l, f * 128:(f + 1) * 128, :], writes=[twd[f]])
        gam, tgam = load_bcast(k, es, "f_gam", k.w["ln2_g"][l])
        bet, tbet = load_bcast(k, es, "f_bet", k.w["ln2_b"][l])
        xt = [_sb(es, nc, "f_xt%d" % i, [128, nj, D], F32) for i in range(2)]; txt = [T("xt%d" % i) for i in range(2)]
        xb = _sb(es, nc, "f_xb", [128, nj, D], BF16); txb = T("xb")
        xT = _sb(es, nc, "f_xT", [128, 8, NB], BF16); txT = [T("xT%d" % i) for i in range(8)]
        hT = _sb(es, nc, "f_hT", [128, NF, NB], BF16); thT = [T("hT%d" % i) for i in range(NF)]
        sg = [_sb(es, nc, "f_sg%d" % i, [128, NB], F32) for i in range(2)]; tsg = [T("sg%d" % i) for i in range(2)]
        y = [_sb(es, nc, "f_y%d" % i, [128, D], F32) for i in range(2)]; ty = [T("y%d" % i) for i in range(2)]
        yo = [_sb(es, nc, "f_yo%d" % i, [128, nj, D], F32) for i in range(2)]; tyo = [T("yo%d" % i) for i in range(2)]
        st = _sb(es, nc, "f_st", [128, 2, 6], F32); tst = T("st")
        mv = _sb(es, nc, "f_mv", [128, 4], F32); tmv = T("mv")
        psT = [_ps(es, nc, "f_psT%d" % i, [128, 1024], BF16) for i in range(2)]; tpsT = [T("psT%d" % i) for i in range(2)]
        psg = [_ps(es, nc, "f_psg%d" % i, [128, 512], F32) for i in range(2)]; tpsg = [T("psg%d" % i) for i in range(2)]
        psu = [_ps(es, nc, "f_psu%d" % i, [128, 512], F32) for i in range(2)]; tpsu = [T("psu%d" % i) for i in range(2)]
        psd = [_ps(es, nc, "f_psd%d" % i, [128, 512], F32) for i in range(2)]; tpsd = [T("psd%d" % i) for i in range(2)]
        nblk = NT // NB
        ctr = [0]

        def load(b):
            S.dma("sp", xt[b % 2][:], k.x1[b * NB:(b + 1) * NB, :].rearrange("(j p) d -> p j d", p=128), writes=[txt[b % 2]])

        load(0)
        pc = 0
        for b in range(nblk):
            bi = b % 2
            if b + 1 < nblk:
                load(b + 1)
            S.op("pool", lambda e: e.tensor_copy(out=xb[:], in_=xt[bi][:]), reads=[txt[bi]], writes=[txb])
            transpose_block(k, xb, txb, xT, txT, ident[:], tid, psT, tpsT, NB, ctr)
            for f in range(NF):
                pi = f % 2
                for kc in range(8):
                    S.op("pe", lambda e, f=f, kc=kc, pi=pi: e.matmul(psg[pi][:, 0:NB], lhsT=wg[:, kc, f * 128:(f + 1) * 128], rhs=xT[:, kc, :], start=(kc == 0), stop=(kc == 7)),
                         reads=[twg[kc], txT[kc]], writes=[tpsg[pi]])
                for kc in range(8):
                    S.op("pe", lambda e, f=f, kc=kc, pi=pi: e.matmul(psu[pi][:, 0:NB], lhsT=wu[:, kc, f * 128:(f + 1) * 128], rhs=xT[:, kc, :], start=(kc == 0), stop=(kc == 7)),
                         reads=[twu[kc], txT[kc]], writes=[tpsu[pi]])
                S.op("act", lambda e, pi=pi: e.activation(out=sg[pi][:], in_=psg[pi][:, 0:NB], func=AF.Silu), reads=[tpsg[pi]], writes=[tsg[pi]])
                S.op("dve", lambda e, f=f, pi=pi: e.tensor_tensor(out=hT[:, f, :], in0=sg[pi][:], in1=psu[pi][:, 0:NB], op=ALU.mult), reads=[tsg[pi], tpsu[pi]], writes=[thT[f]])
            for j in range(nj):
                yi = j % 2
                for half in range(2):
                    pi = pc % 2
                    pc += 1
                    for f in range(NF):
                        S.op("pe", lambda e, j=j, f=f, pi=pi, half=half: e.matmul(psd[pi][:], lhsT=hT[:, f, j * 128:(j + 1) * 128], rhs=wd[:, f, half * 512:(half + 1) * 512], start=(f == 0), stop=(f == NF - 1)),
                             reads=[thT[f], twd[f]], writes=[tpsd[pi]])
                    S.op("dve", lambda e, j=j, pi=pi, half=half, yi=yi: e.scalar_tensor_tensor(out=y[yi][:, half * 512:(half + 1) * 512], in0=xt[bi][:, j, half * 512:(half + 1) * 512], scalar=ALPHA, in1=psd[pi][:], op0=ALU.mult, op1=ALU.add),
                         reads=[txt[bi], tpsd[pi]], writes=[ty[yi]])
                layer_norm_rows(k, y[yi][:], ty[yi], yo[bi][:, j, :], tyo[bi], gam[:], tgam, bet[:], tbet, st, tst, mv, tmv)
            S.dma("sp", xout[b * NB:(b + 1) * NB, :].rearrange("(j p) d -> p j d", p=128), yo[bi][:], reads=[tyo[bi]])
        S.barrier()


def kernel(**inputs):
    x = np.ascontiguousarray(np.asarray(inputs["x"], dtype=np.float32))
    B = x.shape[0]
    nseq = B // N_CORES
    consts = _consts(np.asarray(inputs["rel_bias"], dtype=np.float32))
    kk = build(nseq=nseq, depth=DEPTH)
    base = {n: np.ascontiguousarray(np.asarray(inputs[n], dtype=np.float32)) for n in kk.wnames}
    for n, v in consts.items():
        base["c_" + n] = np.ascontiguousarray(v.astype(np.float32))
    in_maps = []
    for c in range(N_CORES):
        m = dict(base)
        m["x"] = x[c * nseq:(c + 1) * nseq].reshape(nseq * SEQ, D)
        in_maps.append(m)
    res = run_bass_kernel_spmd(kk.nc, in_maps, core_ids=list(range(N_CORES)))
    outs = [np.asarray(r["out"]).reshape(nseq, SEQ, D) for r in res.results]
    return np.concatenate(outs, axis=0).astype(np.float32)
```
